# Optimizing a Trainium2 kernel written in Bass

```python
import jax, jax.numpy as jnp
from jax import lax
import numpy as np

D_MODEL = 1024
BATCH = 4
SEQ = 8192
DEPTH = 2

GRID_W = 64
CTX_LEN = 256
HEAD_DIM = 64
EPS = 1e-6
A_HEADS = 4
A_WIDTH = A_HEADS * HEAD_DIM
CHUNK = 128
B_GROUPS = 4
B_WIDTH = B_GROUPS * HEAD_DIM
C_Q_HEADS = 8
C_KV_HEADS = 2
C_GROUP = C_Q_HEADS // C_KV_HEADS
C_WIDTH = C_Q_HEADS * HEAD_DIM
KV_WIDTH = C_KV_HEADS * HEAD_DIM
WINDOW = 128
BLOCK = 128
ROPE_BASE = 10000.0
D_MIX = A_WIDTH + B_WIDTH + C_WIDTH
OFF_B = 2 * A_WIDTH
OFF_Q = OFF_B + B_WIDTH
OFF_K = OFF_Q + C_WIDTH
OFF_V = OFF_K + KV_WIDTH
N_IN = OFF_V + KV_WIDTH
D_FF = 3584
N_EXPERTS = 8
TOP_K = 2
N_DENSE = (DEPTH + 1) // 2
N_MOE = DEPTH // 2

kernel_name = "hybrid_parallel_mixer_diffusion_block"


def rmsnorm(x, g):
    xf = x.astype(jnp.float32)
    y = xf * lax.rsqrt(jnp.mean(xf * xf, axis=-1, keepdims=True) + EPS)
    return (y * g.astype(jnp.float32)).astype(x.dtype)


def adaln(cond, w, b):
    m = jax.nn.silu(cond) @ w + b
    return jnp.split(m[..., None, :], 6, axis=-1)


def modulate(y, shift, scale):
    return y * (1 + scale) + shift


def axial_rope(n_tok):
    rows = n_tok // GRID_W
    row = jnp.broadcast_to(jnp.arange(rows)[:, None], (rows, GRID_W)).reshape(-1)
    col = jnp.broadcast_to(jnp.arange(GRID_W)[None, :], (rows, GRID_W)).reshape(-1)
    half = HEAD_DIM // 2
    inv = ROPE_BASE ** (-jnp.arange(0, half, 2, dtype=jnp.float32) / half)
    ang = jnp.stack([row.astype(jnp.float32)[:, None] * inv,
                     col.astype(jnp.float32)[:, None] * inv], axis=1)
    return jnp.cos(ang), jnp.sin(ang)


def apply_rope(x, cos, sin):
    xf = x.astype(jnp.float32).reshape(x.shape[:-1] + (2, 2, HEAD_DIM // 4))
    x1, x2 = xf[..., 0, :], xf[..., 1, :]
    cs, sn = cos[:, None], sin[:, None]
    out = jnp.stack([x1 * cs - x2 * sn, x2 * cs + x1 * sn], axis=-2)
    return out.reshape(x.shape).astype(x.dtype)


def chunk_sgu(uv, w_s, b_s, g_v):
    bsz, n, _ = uv.shape
    u, v = jnp.split(uv, 2, axis=-1)
    v = rmsnorm(v.reshape(bsz, n, A_HEADS, HEAD_DIM), g_v)
    v = v.reshape(bsz, n // CHUNK, CHUNK, A_HEADS, HEAD_DIM)
    sv = jnp.einsum('hpq,bcqhd->bcphd', w_s, v) + b_s.T[None, None, :, :, None]
    return u * sv.reshape(bsz, n, A_WIDTH)


def fourier_mix(xb, w_f):
    bsz, n, _ = xb.shape
    xg = xb.astype(jnp.float32).reshape(bsz, n, B_GROUPS, HEAD_DIM)
    y = jnp.fft.fft2(xg, axes=(1, 3), norm='ortho').real.astype(xb.dtype)
    y = jnp.einsum('bngd,gde->bnge', y, w_f)
    return y.reshape(bsz, n, B_WIDTH)


def window_attention(q, k, v, kc, vc, sink):
    bsz, n = q.shape[:2]
    nb = n // BLOCK
    scale = HEAD_DIM ** -0.5
    qb = q.reshape(bsz, nb, BLOCK, C_KV_HEADS, C_GROUP, HEAD_DIM)

    def band(t):
        tb = jnp.pad(t, ((0, 0), (BLOCK, BLOCK), (0, 0), (0, 0)))
        tb = tb.reshape(bsz, nb + 2, BLOCK, C_KV_HEADS, HEAD_DIM)
        return jnp.concatenate([tb[:, :-2], tb[:, 1:-1], tb[:, 2:]], axis=2)

    kb, vb = band(k), band(v)
    s_loc = jnp.einsum('bnqkgd,bnskd->bnkgqs', qb, kb,
                       preferred_element_type=jnp.float32) * scale
    s_ctx = jnp.einsum('bnqkgd,bckd->bnkgqc', qb, kc,
                       preferred_element_type=jnp.float32) * scale
    kpos = jnp.arange(3 * BLOCK) - BLOCK
    rel = kpos[None, :] - jnp.arange(BLOCK)[:, None]
    gpos = jnp.arange(nb)[:, None, None] * BLOCK + kpos[None, None, :]
    valid = (jnp.abs(rel) <= WINDOW)[None] & (gpos >= 0) & (gpos < n)
    s_loc = jnp.where(valid[None, :, None, None], s_loc, -jnp.inf)
    sink_col = jnp.broadcast_to(
        sink.astype(jnp.float32).reshape(C_KV_HEADS, C_GROUP, 1, 1), s_loc.shape[:-1] + (1,))
    p = jax.nn.softmax(jnp.concatenate([s_loc, s_ctx, sink_col], axis=-1), axis=-1)
    n_loc = 3 * BLOCK
    n_ctx = kc.shape[1]
    p_loc = p[..., :n_loc].astype(v.dtype)
    p_ctx = p[..., n_loc:n_loc + n_ctx].astype(v.dtype)
    o = (jnp.einsum('bnkgqs,bnskd->bnqkgd', p_loc, vb)
         + jnp.einsum('bnkgqc,bckd->bnqkgd', p_ctx, vc))
    return o.reshape(bsz, n, C_WIDTH)


def context_attention(qc, kc, vc, sink):
    bsz, m = qc.shape[:2]
    qg = qc.reshape(bsz, m, C_KV_HEADS, C_GROUP, HEAD_DIM)
    s = jnp.einsum('bqkgd,bckd->bkgqc', qg, kc,
                   preferred_element_type=jnp.float32) * HEAD_DIM ** -0.5
    sink_col = jnp.broadcast_to(
        sink.astype(jnp.float32).reshape(C_KV_HEADS, C_GROUP, 1, 1), s.shape[:-1] + (1,))
    p = jax.nn.softmax(jnp.concatenate([s, sink_col], axis=-1), axis=-1)
    o = jnp.einsum('bkgqc,bckd->bqkgd', p[..., :-1].astype(vc.dtype), vc)
    return o.reshape(bsz, m, C_WIDTH)


def swiglu(y, wg, wu, wd):
    return (jax.nn.silu(y @ wg) * (y @ wu)) @ wd


def moe_swiglu(y, w_r, b_r, wg, wu, wd):
    logits = (y @ w_r).astype(jnp.float32) + b_r.astype(jnp.float32)
    top_val, top_idx = lax.top_k(logits, TOP_K)
    gates = jax.nn.softmax(top_val, axis=-1)
    combine = jnp.sum(jax.nn.one_hot(top_idx, N_EXPERTS, dtype=jnp.float32)
                      * gates[..., None], axis=-2).astype(y.dtype)
    out = jnp.zeros_like(y)
    for e in range(N_EXPERTS):
        out = out + combine[..., e:e + 1] * swiglu(y, wg[e], wu[e], wd[e])
    return out


def setup_inputs(seed: int = 0) -> dict:
    key = jax.random.key(seed)
    ks = jax.random.split(key, 25)

    def nrm(k, shape, s):
        return jax.random.normal(k, shape, jnp.float32) * s

    return {
        "x": nrm(ks[0], (BATCH, SEQ, D_MODEL), 1.0),
        "c": nrm(ks[1], (BATCH, D_MODEL), 1.0),
        "ctx": nrm(ks[2], (BATCH, CTX_LEN, D_MODEL), 1.0),
        "c_ctx": nrm(ks[3], (D_MODEL,), 1.0),
        "w_ada": nrm(ks[4], (DEPTH, D_MODEL, 6 * D_MODEL), 0.5 * D_MODEL ** -0.5),
        "b_ada": nrm(ks[5], (DEPTH, 6 * D_MODEL), 0.02),
        "g_mix_pre": 1.0 + nrm(ks[6], (DEPTH, D_MODEL), 0.1),
        "g_mix_post": 1.0 + nrm(ks[7], (DEPTH, D_MODEL), 0.1),
        "g_ffn_pre": 1.0 + nrm(ks[8], (DEPTH, D_MODEL), 0.1),
        "g_ffn_post": 1.0 + nrm(ks[9], (DEPTH, D_MODEL), 0.1),
        "w_in": nrm(ks[10], (DEPTH, D_MODEL, N_IN), D_MODEL ** -0.5),
        "w_s": nrm(ks[11], (DEPTH, A_HEADS, CHUNK, CHUNK), CHUNK ** -0.5),
        "b_s": 1.0 + nrm(ks[12], (DEPTH, A_HEADS, CHUNK), 0.1),
        "g_v": 1.0 + nrm(ks[13], (DEPTH, A_HEADS, HEAD_DIM), 0.1),
        "w_f": nrm(ks[14], (DEPTH, B_GROUPS, HEAD_DIM, HEAD_DIM), HEAD_DIM ** -0.5),
        "sink": nrm(ks[15], (DEPTH, C_Q_HEADS), 0.5),
        "w_out": nrm(ks[16], (DEPTH, D_MIX, D_MODEL), D_MIX ** -0.5),
        "w_gate_d": nrm(ks[17], (N_DENSE, D_MODEL, D_FF), D_MODEL ** -0.5),
        "w_up_d": nrm(ks[18], (N_DENSE, D_MODEL, D_FF), D_MODEL ** -0.5),
        "w_down_d": nrm(ks[19], (N_DENSE, D_FF, D_MODEL), D_FF ** -0.5),
        "w_router": nrm(ks[20], (N_MOE, D_MODEL, N_EXPERTS), D_MODEL ** -0.5),
        "b_router": nrm(ks[21], (N_MOE, N_EXPERTS), 0.01),
        "w_gate_e": nrm(ks[22], (N_MOE, N_EXPERTS, D_MODEL, D_FF), D_MODEL ** -0.5),
        "w_up_e": nrm(ks[23], (N_MOE, N_EXPERTS, D_MODEL, D_FF), D_MODEL ** -0.5),
        "w_down_e": nrm(ks[24], (N_MOE, N_EXPERTS, D_FF, D_MODEL), D_FF ** -0.5),
    }


def reference(x, c, ctx, c_ctx, w_ada, b_ada, g_mix_pre, g_mix_post, g_ffn_pre, g_ffn_post,
              w_in, w_s, b_s, g_v, w_f, sink, w_out, w_gate_d, w_up_d, w_down_d,
              w_router, b_router, w_gate_e, w_up_e, w_down_e):
    bsz, n_lat = x.shape[:2]
    n_ctx = ctx.shape[1]
    cos, sin = axial_rope(n_lat)
    h, hc = x, ctx
    for i in range(DEPTH):
        last = i == DEPTH - 1
        sh_m, sc_m, gt_m, sh_f, sc_f, gt_f = adaln(c, w_ada[i], b_ada[i])
        csh_m, csc_m, cgt_m, csh_f, csc_f, cgt_f = adaln(c_ctx[None, :], w_ada[i], b_ada[i])

        z = modulate(rmsnorm(h, g_mix_pre[i]), sh_m, sc_m) @ w_in[i]
        a, fb, q, k, v = jnp.split(z, [OFF_B, OFF_Q, OFF_K, OFF_V], axis=-1)
        q = apply_rope(q.reshape(bsz, n_lat, C_Q_HEADS, HEAD_DIM), cos, sin)
        k = apply_rope(k.reshape(bsz, n_lat, C_KV_HEADS, HEAD_DIM), cos, sin)
        v = v.reshape(bsz, n_lat, C_KV_HEADS, HEAD_DIM)
        zc_in = modulate(rmsnorm(hc, g_mix_pre[i]), csh_m, csc_m)
        kc, vc = jnp.split(zc_in @ w_in[i][:, OFF_K:], 2, axis=-1)
        kc = kc.reshape(bsz, n_ctx, C_KV_HEADS, HEAD_DIM)
        vc = vc.reshape(bsz, n_ctx, C_KV_HEADS, HEAD_DIM)
        o = jnp.concatenate([
            chunk_sgu(jax.nn.gelu(a), w_s[i], b_s[i], g_v[i]),
            fourier_mix(fb, w_f[i]),
            window_attention(q, k, v, kc, vc, sink[i]),
        ], axis=-1) @ w_out[i]
        h = h + gt_m * rmsnorm(o, g_mix_post[i])
        if not last:
            ac, fbc, qc = jnp.split(zc_in @ w_in[i][:, :OFF_K], [OFF_B, OFF_Q], axis=-1)
            oc = jnp.concatenate([
                chunk_sgu(jax.nn.gelu(ac), w_s[i], b_s[i], g_v[i]),
                fourier_mix(fbc, w_f[i]),
                context_attention(qc.reshape(bsz, n_ctx, C_Q_HEADS, HEAD_DIM), kc, vc, sink[i]),
            ], axis=-1) @ w_out[i]
            hc = hc + cgt_m * rmsnorm(oc, g_mix_post[i])

        j = i // 2
        if i % 2 == 0:
            def ffn(y):
                return swiglu(y, w_gate_d[j], w_up_d[j], w_down_d[j])
        else:
            def ffn(y):
                return moe_swiglu(y, w_router[j], b_router[j], w_gate_e[j], w_up_e[j], w_down_e[j])
        f = ffn(modulate(rmsnorm(h, g_ffn_pre[i]), sh_f, sc_f))
        h = h + gt_f * rmsnorm(f, g_ffn_post[i])
        if not last:
            fc = ffn(modulate(rmsnorm(hc, g_ffn_pre[i]), csh_f, csc_f))
            hc = hc + cgt_f * rmsnorm(fc, g_ffn_post[i])
    return h
```

```python
import numpy as np
import ml_dtypes
import concourse.bass as bass
import concourse.mybir as mybir
from concourse.bass_utils import run_bass_kernel_spmd

F32 = mybir.dt.float32
BF16 = mybir.dt.bfloat16
I32 = mybir.dt.int32
AF = mybir.ActivationFunctionType
ALU = mybir.AluOpType
AX = mybir.AxisListType

EPOCH = 30000
DMA_RING = 24


class Buf:
    __slots__ = ("name", "ap", "t", "last_w", "readers", "excl")

    def __init__(self, name, t):
        self.name = name
        self.t = t
        self.ap = t.ap() if hasattr(t, "ap") and callable(t.ap) else t
        self.last_w = None
        self.readers = {}
        self.excl = False


class Ctx:
    ENG = ("pe", "act", "dve", "pool", "sp")

    def __init__(self, nc):
        self.nc = nc
        self.prog = {e: [] for e in self.ENG}
        self.cnt = {e: 0 for e in self.ENG}
        self.sem = {}
        self.nsem = 0
        for e in ("pe", "act", "dve", "pool"):
            self.sem[e] = self._new_sem(e)
        self.ring = {"sp": DMA_RING, "pool": 40, "act": 2}
        self.dma_sems = {q: [self._new_sem("d" + q) for _ in range(self.ring[q])] for q in ("sp", "pool", "act")}
        self.dma_k = {q: 0 for q in ("sp", "pool", "act")}
        self.known = {e: {} for e in self.ENG}
        self.out_events = []
        self.n_ops = 0
        self.pending = {}

    def _new_sem(self, tag):
        self.nsem += 1
        return self.nc.alloc_semaphore(name="s_%s_%d" % (tag, self.nsem))

    def dram_in(self, name, shape, dt):
        return Buf(name, self.nc.dram_tensor(name, list(shape), dt, kind="ExternalInput"))

    def dram_out(self, name, shape, dt):
        return Buf(name, self.nc.dram_tensor(name, list(shape), dt, kind="ExternalOutput"))

    def dram(self, name, shape, dt):
        return Buf(name, self.nc.dram_tensor(name, list(shape), dt, kind="Internal"))

    def sb(self, name, shape, dt):
        return Buf(name, self.nc.alloc_sbuf_tensor(name, list(shape), dt))

    def ps(self, name, shape, dt=F32):
        b = Buf(name, self.nc.alloc_psum_tensor(name, list(shape), dt))
        b.excl = True
        return b

    def _deps(self, reads, writes):
        deps = {}

        def add(ev):
            if ev is None:
                return
            s, v = ev
            k = id(s)
            if k not in deps or deps[k][1] < v:
                deps[k] = (s, v)
        for b in reads:
            add(b.last_w)
        for b in writes:
            add(b.last_w)
            for ev in b.readers.values():
                add(ev)
        return list(deps.values())

    def _record(self, ev, reads, writes):
        for b in reads:
            if b in writes:
                continue
            b.readers[id(ev[0])] = ev
        for b in writes:
            b.last_w = ev
            b.readers = {}

    def _waits(self, eng, deps):
        kn = self.known[eng]
        out = []
        for s, v in deps:
            k = id(s)
            if kn.get(k, 0) >= v:
                continue
            kn[k] = v
            out.append((s, v))
        return out

    def _all_events(self):
        evs = []
        for q in self.dma_sems:
            k = self.dma_k[q]
            R_ = self.ring[q]
            for j in range(min(k, R_)):
                n_on_j = (k - 1 - j) // R_ + 1
                evs.append((self.dma_sems[q][j], 16 * n_on_j))
        for e in ("pe", "act", "dve", "pool"):
            if self.cnt[e] > 0:
                evs.append((self.sem[e], self.cnt[e]))
        return evs

    def barrier(self):
        evs = self._all_events()
        for e in self.ENG:
            self.pending[e] = list(evs)

    def op(self, eng, fn, reads=(), writes=(), nosync_same=False):
        ex = [b for b in reads if b.excl]
        if ex:
            writes = list(writes) + [b for b in ex if b not in writes]
        deps = self._deps(reads, writes)
        if nosync_same:
            deps = [d for d in deps if d[0] is not self.sem[eng]]
        deps += self.pending.pop(eng, [])
        waits = self._waits(eng, deps)
        if self.cnt[eng] >= EPOCH:
            self.sem[eng] = self._new_sem(eng)
            self.cnt[eng] = 0
        self.cnt[eng] += 1
        ev = (self.sem[eng], self.cnt[eng])
        self.prog[eng].append((waits, fn, ev[0], 1))
        self._record(ev, reads, writes)
        self.n_ops += 1
        return ev

    def dma(self, q, dst_buf, out_ap, in_ap, reads=(), writes=None, **kw):
        if writes is None:
            writes = [dst_buf]
        deps = self._deps(reads, writes)
        k = self.dma_k[q]
        self.dma_k[q] += 1
        R_ = self.ring[q]
        s = self.dma_sems[q][k % R_]
        v = 16 * (k // R_ + 1)
        if k >= R_:
            deps.append((s, v - 16))
        deps += self.pending.pop(q, [])
        waits = self._waits(q, deps)
        ev = (s, v)
        self.prog[q].append((waits, (lambda e, o=out_ap, i=in_ap, kw=kw: e.dma_start(out=o, in_=i, **kw)), s, 16))
        self._record(ev, reads, writes)
        self.n_ops += 1
        return ev

    def dma_custom(self, q, fn, reads=(), writes=()):
        deps = self._deps(reads, writes)
        k = self.dma_k[q]
        self.dma_k[q] += 1
        R_ = self.ring[q]
        s = self.dma_sems[q][k % R_]
        v = 16 * (k // R_ + 1)
        if k >= R_:
            deps.append((s, v - 16))
        deps += self.pending.pop(q, [])
        waits = self._waits(q, deps)
        ev = (s, v)
        self.prog[q].append((waits, fn, s, 16))
        self._record(ev, reads, writes)
        self.n_ops += 1
        return ev

    def mark_output(self, ev):
        self.out_events.append(ev)

    def finish(self, out_bufs=()):
        finals = list(self.out_events) + self._all_events()
        final_waits = self._waits("sp", finals)
        prog = self.prog
        nc = self.nc
        engmap = {"pe": "tensor", "act": "scalar", "dve": "vector", "pool": "gpsimd", "sp": "sync"}

        def replay(name):
            def body(e):
                for waits, fn, s, inc in prog[name]:
                    for ws, wv in waits:
                        e.wait_ge(ws, wv)
                    fn(e).then_inc(s, inc)
                if name == "sp":
                    for ws, wv in final_waits:
                        e.wait_ge(ws, wv)
            return body
        with nc.Block() as block:
            for name in self.ENG:
                getattr(block, engmap[name])(replay(name))


class Cfg:
    def __init__(self, SEQ=8192, D_FF=3584, BATCH=4):
        self.SEQ, self.D_FF, self.BATCH = SEQ, D_FF, BATCH
        self.D = 1024
        self.CTX = 256
        self.NE = 8
        self.NT = SEQ // 128
        self.NH = self.NT // 2
        self.NFC = D_FF // 128
        self.FG = 4
        assert self.NFC % self.FG == 0
        self.NCORES = 2 * BATCH


REC_W = 832


class _Stop(Exception):
    pass


STOP_AT = None
SPARSE_MOE = True
PRECONV = True


def build(cfg):
    def chk(tag):
        if STOP_AT == tag:
            raise _Stop()
    nc = bass.Bass("TRN2", target_bir_lowering=False)
    c = Ctx(nc)
    D, NT, NH, DFF, NFC, FG, NE = cfg.D, cfg.NT, cfg.NH, cfg.D_FF, cfg.NFC, cfg.FG, cfg.NE
    SEQ = cfg.SEQ
    NG = NFC // FG

    di = c.dram_in
    xb = di("xb", [SEQ, D], F32)
    ctxb = di("ctxb", [256, D], F32)
    cvecT = di("cvecT", [128, 8, 2], F32)
    w_ada = di("w_ada", [2, D, 6 * D], F32)
    badaT = di("badaT", [2, 128, 48], F32)
    gpreT = di("gpreT", [2, 128, 2, 8], F32)
    gpost = di("gpost", [2, 2, 128, D], F32)
    w_in = di("w_in", [2, D, 1536], F32)
    wsT = di("wsT", [2, 4, 128, 128], F32)
    bsT = di("bsT", [2, 128, 4], F32)
    gvb = di("gvb", [2, 128, 256], F32)
    wf = di("wf", [2, 256, 64], F32)
    sinkb = di("sinkb", [2, 128, 8], F32)
    w_out = di("w_out", [2, D, D], F32)
    wg_d = di("wg_d", [D, DFF], F32)
    wu_d = di("wu_d", [D, DFF], F32)
    wd_d = di("wd_d", [DFF, D], F32)
    w_r = di("w_r", [D, NE], F32)
    b_rb = di("b_rb", [128, NE], F32)
    wg_e = di("wg_e", [NE, D, DFF], F32)
    wu_e = di("wu_e", [NE, D, DFF], F32)
    wd_e = di("wd_e", [NE, DFF, D], F32)
    ident_bd = di("ident_b", [128, 128], BF16)
    ident_fd = di("ident_f", [128, 128], F32)
    ropec = di("ropec", [NT + 1, 128, 32], F32)
    ropes = di("ropes", [NT + 1, 128, 32], F32)
    tab = di("tab", [128, NT, 2, SEQ], BF16)
    tabc = di("tabc", [128, 2, 2, 256], BF16)
    c64d = di("c64bd", [128, 128], F32)
    s64d = di("s64bd", [128, 128], F32)
    masksd = di("masks", [128, 6, 128], BF16)
    out = c.dram_out("out", [SEQ // 2, D], F32)
    NHt = NH
    T_OWN = NH * 128
    NS = 2 * T_OWN // 512 + NE
    RPD = DFF // 512
    ltrid = di("ltri", [128, 128], BF16)
    constWd = di("constW", [128, 8 * NG], F32)
    constDd = di("constD", [128, NFC], F32)
    WGB = c.dram("WGB", [NG, 128, 8, FG * 128], BF16)
    WUB = c.dram("WUB", [NG, 128, 8, FG * 128], BF16)
    WDB = c.dram("WDB", [NG, 128, FG, D], BF16)
    Y2 = c.dram("Y2", [NHt, 128, D], BF16)
    Y2S = c.dram("Y2S", [NS * 512, D], BF16)
    RS = c.dram("RS", [NS * 512, D], F32)

    REC = c.dram("REC", [NT, 128, REC_W], BF16)
    OAT = c.dram("OAT", [NT, 128, 256], BF16)
    HM = c.dram("HM", [NT, 128, D], F32)
    H1 = c.dram("H1", [NT, 128, D], F32)
    HCM = c.dram("HCM", [2, 128, D], F32)
    HC1 = c.dram("HC1", [2, 128, D], F32)

    st = {"off": 16384}

    def sb(name, shape, dt):
        esz = 4 if dt in (F32, I32) else 2
        n = 1
        for s_ in shape[1:]:
            n *= s_
        nbytes = (n * esz + 31) // 32 * 32
        t = nc.alloc_sbuf_tensor_at(name, list(shape), dt, offset=st["off"])
        st["off"] += nbytes
        assert st["off"] <= 16384 + 212000, ("SBUF overflow", name, st["off"])
        return Buf(name, t)

    banks = [c.ps("bank%d" % i, [128, 512], F32) for i in range(8)]

    def bfv(bank):
        return bank.ap.bitcast(BF16)

    ident_b = sb("ident_b", [128, 128], BF16)
    ident_f = sb("ident_f", [128, 128], F32)
    ones_f = sb("ones_f", [128, 128], F32)
    masks = sb("masks", [128, 6, 128], BF16)
    c64 = sb("c64", [128, 128], F32)
    s64 = sb("s64", [128, 128], F32)
    cT = sb("cT", [128, 8, 2], F32)
    c.dma("sp", ident_b, ident_b.ap[:], ident_bd.ap[:])
    c.dma("sp", ident_f, ident_f.ap[:], ident_fd.ap[:])
    c.dma("sp", masks, masks.ap[:], masksd.ap[:])
    c.dma("sp", c64, c64.ap[:], c64d.ap[:])
    c.dma("sp", s64, s64.ap[:], s64d.ap[:])
    c.dma("sp", cT, cT.ap[:], cvecT.ap[:])
    c.op("dve", lambda e: e.memset(ones_f.ap[:], 1.0), writes=[ones_f])
    scT = sb("scT", [128, 8, 2], F32)
    c.op("act", lambda e: e.activation(scT.ap[:], cT.ap[:], AF.Silu), reads=[cT], writes=[scT])

    adaT = sb("adaT", [128, 48, 2], F32)
    bada = sb("bada", [128, 48], F32)
    gpre = sb("gpre", [128, 2, 8], F32)
    MUL = sb("MUL", [128, 2, 2, 8], F32)
    G = [[sb("G%d%d" % (a, b), [128, D], F32) for b in range(2)] for a in range(2)]
    wsTb = sb("wsTb", [128, 4, 128], BF16)
    bsTs = sb("bsTs", [128, 4], F32)
    gvs = sb("gvs", [128, 256], F32)
    CW = sb("CW", [128, 2, 2, 128], BF16)
    esink = sb("esink", [128, 512], F32)
    recC = [sb("recC%d" % i, [128, REC_W], BF16) for i in range(2)]
    MULbc = sb("MULbc", [128, D], F32)
    ADDbc = sb("ADDbc", [128, D], F32)
    tabc_sb = sb("tabc_sb", [128, 2, 2, 256], BF16)
    c.dma("sp", tabc_sb, tabc_sb.ap[:], tabc.ap[:])
    persistent_end = st["off"]

    def norm_bufs(tag):
        return dict(
            ht=sb(tag + "ht", [128, D], F32),
            junk=sb(tag + "junk", [128, D], BF16),
            ss=sb(tag + "ss", [128, 1], F32),
            rs=sb(tag + "rs", [128, 1], F32),
            xn=sb(tag + "xn", [128, D], BF16),
        )

    def emit_norm_T(nb, who, which, ymT_ap_fn, ymT_buf, tbank):
        ht, junk, ss, rs, xn = nb["ht"], nb["junk"], nb["ss"], nb["rs"], nb["xn"]
        c.op("pool", lambda e: e.memset(ss.ap[:], 0.0), writes=[ss])
        c.op("act", lambda e: e.activation(junk.ap[:], ht.ap[:], AF.Square, accum_out=ss.ap[:, 0:1]),
             reads=[ht], writes=[junk, ss])
        c.op("act", lambda e: e.activation(rs.ap[:], ss.ap[:], AF.Sqrt, bias=1e-6, scale=1.0 / D),
             reads=[ss], writes=[rs])
        c.op("dve", lambda e: e.reciprocal(rs.ap[:], rs.ap[:]), reads=[rs], writes=[rs])
        c.op("dve", lambda e: e.tensor_scalar(xn.ap[:], ht.ap[:], rs.ap[:, 0:1], None, ALU.mult),
             reads=[ht, rs], writes=[xn])
        tv = bfv(tbank)
        for ci in range(8):
            c.op("pe", lambda e, ci=ci: e.transpose(tv[:, ci * 128:(ci + 1) * 128], xn.ap[:, ci * 128:(ci + 1) * 128], ident_b.ap[:]),
                 reads=[xn, ident_b], writes=[tbank], nosync_same=True)
        for ci in range(8):
            eng = "dve" if ci % 2 == 0 else "act"
            mul_ap = MUL.ap[:, which, who, ci:ci + 1]
            add_ap = adaT.ap[:, (0 if which == 0 else 3) * 8 + ci, who:who + 1]
            if eng == "dve":
                c.op("dve", lambda e, ci=ci, m=mul_ap, a=add_ap: e.tensor_scalar(
                    ymT_ap_fn(ci), tv[:, ci * 128:(ci + 1) * 128], m, a, ALU.mult, ALU.add),
                    reads=[tbank, MUL, adaT], writes=[ymT_buf])
            else:
                c.op("act", lambda e, ci=ci, m=mul_ap, a=add_ap: e.activation(
                    ymT_ap_fn(ci), tv[:, ci * 128:(ci + 1) * 128], AF.Identity, bias=a, scale=m),
                    reads=[tbank, MUL, adaT], writes=[ymT_buf])

    def layer(L):
        last = (L == 1)
        st["off"] = persistent_end
        c.barrier()
        c.dma("sp", bada, bada.ap[:], badaT.ap[L])
        c.dma("sp", gpre, gpre.ap[:], gpreT.ap[L])
        wa = [sb("wa%d" % i, [128, 8, 512], F32) for i in range(2)]
        wav = w_ada.ap[L].rearrange("(c p) n -> p c n", p=128)
        pb = banks[0]
        for grp in range(12):
            wb = wa[grp % 2]
            c.dma("sp", wb, wb.ap[:], wav[:, :, grp * 512:(grp + 1) * 512])
            for jj in range(4):
                j = grp * 4 + jj
                for ci in range(8):
                    c.op("pe", lambda e, wb=wb, jj=jj, j=j, ci=ci: e.matmul(
                        pb.ap[:, 2 * j:2 * j + 2], wb.ap[:, ci, jj * 128:(jj + 1) * 128], scT.ap[:, ci, :],
                        start=(ci == 0), stop=(ci == 7)), reads=[wb, scT], writes=[pb], nosync_same=True)
        for v in range(2):
            c.op("dve", lambda e, v=v: e.tensor_tensor(
                adaT.ap[:, :, v], pb.ap[:, 0:96].rearrange("p (j v) -> p j v", v=2)[:, :, v], bada.ap[:], ALU.add),
                reads=[pb, bada], writes=[adaT])
        for which in range(2):
            for who in range(2):
                scv = adaT.ap[:, (1 if which == 0 else 4) * 8:(1 if which == 0 else 4) * 8 + 8, who]
                c.op("dve", lambda e, which=which, who=who, scv=scv: e.scalar_tensor_tensor(
                    MUL.ap[:, which, who, :], scv, 1.0, gpre.ap[:, which, :], ALU.add, ALU.mult),
                    reads=[adaT, gpre], writes=[MUL])
        gp = sb("gp", [128, D], F32)
        bc = sb("bcst", [128, 128], F32)
        for which in range(2):
            c.dma("sp", gp, gp.ap[:], gpost.ap[L, which])
            for who in range(2):
                for ci in range(8):
                    col = adaT.ap[:, (2 if which == 0 else 5) * 8 + ci, who:who + 1]
                    c.op("dve", lambda e, col=col: e.tensor_scalar(bc.ap[:], ones_f.ap[:], col, None, ALU.mult),
                         reads=[ones_f, adaT], writes=[bc])
                    bk = banks[1 + ci // 4]
                    c.op("pe", lambda e, bk=bk, ci=ci: e.matmul(
                        bk.ap[:, (ci % 4) * 128:(ci % 4 + 1) * 128], bc.ap[:], ident_f.ap[:], start=True, stop=True),
                        reads=[bc, ident_f], writes=[bk], nosync_same=True)
                for hh in range(2):
                    c.op("dve", lambda e, hh=hh, which=which, who=who: e.tensor_tensor(
                        G[which][who].ap[:, hh * 512:(hh + 1) * 512], banks[1 + hh].ap[:], gp.ap[:, hh * 512:(hh + 1) * 512], ALU.mult),
                        reads=[banks[1 + hh], gp], writes=[G[which][who]])
        if L == 1 and SPARSE_MOE:
            for dst_t, colfn in ((MULbc, lambda ci: MUL.ap[:, 1, 0, ci:ci + 1]), (ADDbc, lambda ci: adaT.ap[:, 3 * 8 + ci, 0:1])):
                for ci in range(8):
                    col = colfn(ci)
                    c.op("dve", lambda e, col=col: e.tensor_scalar(bc.ap[:], ones_f.ap[:], col, None, ALU.mult),
                         reads=[ones_f, adaT, MUL], writes=[bc])
                    bk = banks[1 + ci // 4]
                    c.op("pe", lambda e, bk=bk, ci=ci: e.matmul(
                        bk.ap[:, (ci % 4) * 128:(ci % 4 + 1) * 128], bc.ap[:], ident_f.ap[:], start=True, stop=True),
                        reads=[bc, ident_f], writes=[bk], nosync_same=True)
                for hh in range(2):
                    c.op("act", lambda e, hh=hh, dst_t=dst_t: e.copy(dst_t.ap[:, hh * 512:(hh + 1) * 512], banks[1 + hh].ap[:]),
                         reads=[banks[1 + hh]], writes=[dst_t])
        for h in range(4):
            c.dma("pool", wsTb, wsTb.ap[:, h, :], wsT.ap[L, h])
        c.dma("sp", bsTs, bsTs.ap[:], bsT.ap[L])
        c.dma("sp", gvs, gvs.ap[:], gvb.ap[L])
        sk = sb("sk", [128, 8], F32)
        c.dma("sp", sk, sk.ap[:], sinkb.ap[L])
        c.op("act", lambda e: e.activation(sk.ap[:], sk.ap[:], AF.Exp), reads=[sk], writes=[sk])
        c.op("dve", lambda e: e.tensor_copy(
            esink.ap[0:64, :].rearrange("p (c q) -> p c q", q=128), sk.ap[0:64, 4:8].unsqueeze(2).broadcast_to([64, 4, 128])),
            reads=[sk], writes=[esink])
        c.op("dve", lambda e: e.tensor_copy(
            esink.ap[64:128, :].rearrange("p (c q) -> p c q", q=128), sk.ap[64:128, 0:4].unsqueeze(2).broadcast_to([64, 4, 128])),
            reads=[sk], writes=[esink])
        wfs = sb("wfs", [128, 2, 64], F32)
        c.dma("sp", wfs, wfs.ap[:], wf.ap[L].rearrange("(j p) e -> p j e", p=128))
        c.op("pool", lambda e: e.memset(CW.ap[:], 0.0), writes=[CW])
        for part, cs in enumerate((c64, s64)):
            for j in range(2):
                bk = banks[3]
                c.op("pe", lambda e, cs=cs, j=j, bk=bk: e.matmul(bk.ap[:, 0:64], cs.ap[:], wfs.ap[:, j, :], start=True, stop=True),
                     reads=[cs, wfs], writes=[bk], nosync_same=True)
                c.op("dve", lambda e, part=part, j=j, bk=bk: e.tensor_copy(CW.ap[0:64, part, j, 0:64], bk.ap[0:64, 0:64]),
                     reads=[bk], writes=[CW])
                c.op("dve", lambda e, part=part, j=j, bk=bk: e.tensor_copy(CW.ap[64:128, part, j, 64:128], bk.ap[64:128, 0:64]),
                     reads=[bk], writes=[CW])
        c.barrier()
        chk("setup%d" % L)
        st["off"] = persistent_end
        mixer_top = st["off"]

        P_all = sb("P_all", [128, NT, 512], BF16)
        P_ctx = sb("P_ctx", [128, 2, 512], BF16)
        oatC = sb("oatC", [128, 2, 256], BF16)
        p12_top = st["off"]
        w_in_sb = sb("w_in_sb", [128, 8, 1536], BF16)
        c.dma("pool", w_in_sb, w_in_sb.ap[:], w_in.ap[L].rearrange("(c p) n -> p c n", p=128))
        nbs = [norm_bufs("p1%d" % i) for i in range(2)]
        NSET = 3
        wk = []
        for i in range(NSET):
            d_ = dict(
                ymT=sb("ymT%d" % i, [128, 8, 128], BF16),
                ga=sb("ga%d" % i, [128, 512], F32),
                sq=sb("sq%d" % i, [128, 256], F32),
                ssv=sb("ssv%d" % i, [128, 4], F32),
                vn=sb("vn%d" % i, [128, 256], BF16),
                oA=sb("oA%d" % i, [128, 256], BF16),
                oAT=sb("oAT%d" % i, [128, 256], BF16),
                fbT=sb("fbT%d" % i, [128, 2, 128], BF16),
                zq=sb("zq%d" % i, [128, 640], F32),
                t1=sb("t1%d" % i, [128, 320], F32),
                t2=sb("t2%d" % i, [128, 320], F32),
                qkr=sb("qkr%d" % i, [128, 640], BF16),
                rec=sb("rec%d" % i, [128, REC_W], BF16),
                rc=sb("rc%d" % i, [128, 32], F32),
                rsn=sb("rsn%d" % i, [128, 32], F32),
            )
            c.op("pool", lambda e, d_=d_: e.memset(d_["rec"].ap[:, 704:768], 1.0), writes=[d_["rec"]])
            wk.append(d_)
        for i in range(2):
            c.op("pool", lambda e, i=i: e.memset(recC[i].ap[:, 704:768], 1.0), writes=[recC[i]])

        cnt = {"i": 0}

        def pass1_tile(src_ap, who, rope_idx, fA, fB, fQ, fKV, P_dst_buf, P_dst_ap, rec_sink, oat_sink):
            par = cnt["i"] % 2
            nb, w = nbs[par], wk[cnt["i"] % NSET]
            cnt["i"] += 1
            ymT = w["ymT"]
            c.dma("sp", nb["ht"], nb["ht"].ap[:], src_ap)
            emit_norm_T(nb, who, 0, lambda ci: ymT.ap[:, ci, :], ymT, banks[0])
            chk("p1a")
            def back():
                if fA:
                    zA = banks[1]
                    for ci in range(8):
                        c.op("pe", lambda e, ci=ci: e.matmul(zA.ap[:], ymT.ap[:, ci, :], w_in_sb.ap[:, ci, 0:512], start=(ci == 0), stop=(ci == 7)),
                             reads=[ymT, w_in_sb], writes=[zA], nosync_same=True)
                    ga = w["ga"]
                    c.op("act", lambda e: e.activation(ga.ap[:], zA.ap[:], AF.Gelu), reads=[zA], writes=[ga])
                    sq, ssv, vn, oA, oAT = w["sq"], w["ssv"], w["vn"], w["oA"], w["oAT"]
                    c.op("pool", lambda e: e.tensor_tensor(sq.ap[:], ga.ap[:, 256:512], ga.ap[:, 256:512], ALU.mult), reads=[ga], writes=[sq])
                    c.op("dve", lambda e: e.tensor_reduce(ssv.ap[:], sq.ap[:].rearrange("p (h d) -> p h d", d=64), AX.X, ALU.add), reads=[sq], writes=[ssv])
                    c.op("act", lambda e: e.activation(ssv.ap[:], ssv.ap[:], AF.Sqrt, bias=1e-6, scale=1.0 / 64), reads=[ssv], writes=[ssv])
                    c.op("dve", lambda e: e.reciprocal(ssv.ap[:], ssv.ap[:]), reads=[ssv], writes=[ssv])
                    for h in range(4):
                        c.op("dve", lambda e, h=h: e.scalar_tensor_tensor(
                            vn.ap[:, h * 64:(h + 1) * 64], ga.ap[:, 256 + h * 64:256 + (h + 1) * 64], ssv.ap[:, h:h + 1],
                            gvs.ap[:, h * 64:(h + 1) * 64], ALU.mult, ALU.mult), reads=[ga, ssv, gvs], writes=[vn])
                    yield
                    svb = banks[5]
                    for h in range(4):
                        c.op("pe", lambda e, h=h: e.matmul(svb.ap[:, h * 64:(h + 1) * 64], wsTb.ap[:, h, :], vn.ap[:, h * 64:(h + 1) * 64], start=True, stop=True),
                             reads=[wsTb, vn], writes=[svb], nosync_same=True)
                    for h in range(4):
                        c.op("dve", lambda e, h=h: e.scalar_tensor_tensor(
                            oA.ap[:, h * 64:(h + 1) * 64], svb.ap[:, h * 64:(h + 1) * 64], bsTs.ap[:, h:h + 1],
                            ga.ap[:, h * 64:(h + 1) * 64], ALU.add, ALU.mult), reads=[svb, bsTs, ga], writes=[oA])
                    tb = banks[7]
                    tvv = bfv(tb)
                    for j in range(2):
                        c.op("pe", lambda e, j=j: e.transpose(tvv[:, 640 + j * 128:640 + (j + 1) * 128], oA.ap[:, j * 128:(j + 1) * 128], ident_b.ap[:]),
                             reads=[oA, ident_b], writes=[tb], nosync_same=True)
                    if oat_sink[0] == "dram":
                        c.op("act", lambda e: e.copy(oAT.ap[:], tvv[:, 640:896]), reads=[tb], writes=[oAT])
                        c.dma("pool", OAT, oat_sink[1], oAT.ap[:], reads=[oAT], writes=[])
                    else:
                        c.op("act", lambda e: e.copy(oat_sink[1], tvv[:, 640:896]), reads=[tb], writes=[oat_sink[2]])
                chk("p1b")
                yield
                if fB:
                    zb = banks[2]
                    fbT = w["fbT"]
                    for j in range(2):
                        for ci in range(8):
                            c.op("pe", lambda e, j=j, ci=ci: e.matmul(
                                zb.ap[:, j * 128:(j + 1) * 128], w_in_sb.ap[:, ci, 512 + j * 128:512 + (j + 1) * 128], ymT.ap[:, ci, :],
                                start=(ci == 0), stop=(ci == 7)), reads=[ymT, w_in_sb], writes=[zb], nosync_same=True)
                    c.op("act", lambda e: e.copy(fbT.ap[:].rearrange("p j t -> p (j t)"), zb.ap[:, 0:256]), reads=[zb], writes=[fbT])
                    pbk = banks[6]
                    for part in range(2):
                        for j in range(2):
                            c.op("pe", lambda e, part=part, j=j: e.matmul(
                                pbk.ap[:, part * 256 + j * 128:part * 256 + (j + 1) * 128], fbT.ap[:, j, :], CW.ap[:, part, j, :], start=True, stop=True),
                                reads=[fbT, CW], writes=[pbk], nosync_same=True)
                    c.op("dve", lambda e: e.tensor_copy(P_dst_ap, pbk.ap[:]), reads=[pbk], writes=[P_dst_buf])
                chk("p1c")
                if fQ or fKV:
                    rec = w["rec"] if rec_sink[0] == "dram" else rec_sink[1]
                    zq, t1, t2, qkr = w["zq"], w["t1"], w["t2"], w["qkr"]
                    rc, rsn = w["rc"], w["rsn"]
                    c.dma("sp", rc, rc.ap[:], ropec.ap[rope_idx])
                    c.dma("sp", rsn, rsn.ap[:], ropes.ap[rope_idx])
                    chk("p1d0")
                    b3, b4 = banks[3], banks[4]
                    lo = 0 if fQ else 512
                    if fQ:
                        for ci in range(8):
                            c.op("pe", lambda e, ci=ci: e.matmul(b3.ap[:], ymT.ap[:, ci, :], w_in_sb.ap[:, ci, 768:1280], start=(ci == 0), stop=(ci == 7)),
                                 reads=[ymT, w_in_sb], writes=[b3], nosync_same=True)
                        c.op("act", lambda e: e.copy(zq.ap[:, 0:512], b3.ap[:]), reads=[b3], writes=[zq])
                    chk("p1d1")
                    for ci in range(8):
                        c.op("pe", lambda e, ci=ci: e.matmul(b4.ap[:, 0:256], ymT.ap[:, ci, :], w_in_sb.ap[:, ci, 1280:1536], start=(ci == 0), stop=(ci == 7)),
                             reads=[ymT, w_in_sb], writes=[b4], nosync_same=True)
                    c.op("act", lambda e: e.copy(zq.ap[:, 512:640], b4.ap[:, 0:128]), reads=[b4], writes=[zq])
                    chk("p1d2")
                    c.op("dve", lambda e: e.tensor_copy(rec.ap[:, 640:704], b4.ap[:, 128:192]), reads=[b4], writes=[rec])
                    c.op("dve", lambda e: e.tensor_copy(rec.ap[:, 768:832], b4.ap[:, 192:256]), reads=[b4], writes=[rec])
                    chk("p1d")
                    yield
                    nh_ = (640 - lo) // 64

                    def v5(ap_, half):
                        return ap_.rearrange("p (h a f r) -> p h a f r", a=2, f=2, r=16)[:, :, :, half, :]

                    def tv(ap_):
                        return ap_.rearrange("p (h a r) -> p h a r", a=2, r=16)

                    def tb_(ap_):
                        return ap_.rearrange("p (a r) -> p a r", r=16).unsqueeze(1).broadcast_to([128, nh_, 2, 16])
                    x1 = v5(zq.ap[:, lo:640], 0)
                    x2 = v5(zq.ap[:, lo:640], 1)
                    o1 = v5(qkr.ap[:, lo:640], 0)
                    o2 = v5(qkr.ap[:, lo:640], 1)
                    T1 = tv(t1.ap[:, 0:nh_ * 32])
                    T2 = tv(t2.ap[:, 0:nh_ * 32])
                    cs_, sn_ = tb_(rc.ap[:]), tb_(rsn.ap[:])
                    c.op("dve", lambda e: e.tensor_tensor(T1, x1, cs_, ALU.mult), reads=[zq, rc], writes=[t1])
                    c.op("dve", lambda e: e.tensor_tensor(T2, x2, sn_, ALU.mult), reads=[zq, rsn], writes=[t2])
                    c.op("dve", lambda e: e.tensor_tensor(o1, T1, T2, ALU.subtract), reads=[t1, t2], writes=[qkr])
                    c.op("dve", lambda e: e.tensor_tensor(T1, x2, cs_, ALU.mult), reads=[zq, rc, qkr], writes=[t1])
                    c.op("dve", lambda e: e.tensor_tensor(T2, x1, sn_, ALU.mult), reads=[zq, rsn, qkr], writes=[t2])
                    c.op("dve", lambda e: e.tensor_tensor(o2, T1, T2, ALU.add), reads=[t1, t2], writes=[qkr])
                    chk("p1e")
                    tb = banks[7]
                    tvv = bfv(tb)
                    for j in range(lo // 128, 5):
                        c.op("pe", lambda e, j=j: e.transpose(tvv[:, j * 128:(j + 1) * 128], qkr.ap[:, j * 128:(j + 1) * 128], ident_b.ap[:]),
                             reads=[qkr, ident_b], writes=[tb], nosync_same=True)
                    c.op("act", lambda e: e.copy(rec.ap[:, lo:640], tvv[:, lo:640]), reads=[tb], writes=[rec])
                    if rec_sink[0] == "dram":
                        c.dma("pool", REC, rec_sink[1], rec.ap[:], reads=[rec], writes=[])
                    chk("p1f")
            return back()

        active = []

        def exhaust(g):
            for _ in g:
                pass

        def p1(*a):
            while len(active) >= NSET - 1:
                exhaust(active.pop(0))
            active.append(pass1_tile(*a))
            for g in list(active):
                try:
                    next(g)
                except StopIteration:
                    active.remove(g)
        for i in range(2):
            src = ctxb.ap[i * 128:(i + 1) * 128, :] if L == 0 else HC1.ap[i]
            if L == 0:
                p1(src, 1, NT, True, True, True, True, P_ctx, P_ctx.ap[:, i, :], ("sb", recC[i]), ("sb", oatC.ap[:, i, :], oatC))
            else:
                p1(src, 1, NT, False, False, False, True, None, None, ("sb", recC[i]), None)
        conv_jobs = []
        if L == 0 and PRECONV:
            cvb = [sb("cvb%d" % i, [128, 8, FG * 128], BF16) for i in range(1)]
            cvs = {"k": 0}

            def mk_job(kind, gi):
                def job():
                    b_ = cvb[0]
                    cvs["k"] += 1
                    cols = slice(gi * FG * 128, (gi + 1) * FG * 128)
                    if kind == 0:
                        c.dma("pool", b_, b_.ap[:], wg_d.ap.rearrange("(c p) n -> p c n", p=128)[:, :, cols])
                        c.dma("pool", WGB, WGB.ap[gi], b_.ap[:], reads=[b_], writes=[])
                    elif kind == 1:
                        c.dma("pool", b_, b_.ap[:], wu_d.ap.rearrange("(c p) n -> p c n", p=128)[:, :, cols])
                        c.dma("pool", WUB, WUB.ap[gi], b_.ap[:], reads=[b_], writes=[])
                    else:
                        bv = b_.ap[:].rearrange("p c n -> p (c n)").rearrange("p (f n) -> p f n", n=D)
                        c.dma("pool", b_, bv, wd_d.ap[gi * FG * 128:(gi + 1) * FG * 128, :].rearrange("(c p) n -> p c n", p=128))
                        c.dma("pool", WDB, WDB.ap[gi], bv, reads=[b_], writes=[])
                return job
            for gi in range(NG):
                for kind in range(3):
                    conv_jobs.append(mk_job(kind, gi))
        for t in range(NT):
            if conv_jobs and t % 2 == 0:
                conv_jobs.pop(0)()
            src = xb.ap[t * 128:(t + 1) * 128, :] if L == 0 else H1.ap[t]
            if L == 0 or t < NH:
                fl = (True, True, True, True)
            elif t == NH or t == NT - 1:
                fl = (False, True, False, True)
            else:
                fl = (False, True, False, False)
            p1(src, 0, t, fl[0], fl[1], fl[2], fl[3], P_all, P_all.ap[:, t, :], ("dram", REC.ap[t]), ("dram", OAT.ap[t]))
        while active:
            exhaust(active.pop(0))
        while conv_jobs:
            conv_jobs.pop(0)()
        c.barrier()
        chk("pass1_%d" % L)
        st["off"] = p12_top

        woAB = sb("woAB", [128, 4, D], BF16)
        woC = sb("woC", [128, 4, D], BF16)
        c.dma("pool", woAB, woAB.ap[:], w_out.ap[L, 0:512, :].rearrange("(c p) n -> p c n", p=128))
        for g in range(2):
            c.dma("pool", woC, woC.ap[64 * g:64 * g + 64, :, :],
                  w_out.ap[L, 512 + 256 * g:512 + 256 * g + 256, :].rearrange("(c d) n -> d c n", d=64))
        TG = 4
        tbufs = [sb("tbuf%d" % i, [128, TG, 2, 512], BF16) for i in range(4)]
        oBT = [sb("oBT%d" % i, [128, 2, 512], BF16) for i in range(2)]
        ring = [sb("ring%d" % i, [128, REC_W], BF16) for i in range(5)]
        oats = [sb("oats%d" % i, [128, 256], BF16) for i in range(2)]
        pTs = [sb("pT%d" % i, [128, 512], BF16) for i in range(3)]
        dn = [sb("dn%d" % i, [128, 512], F32) for i in range(2)]
        rcp = [sb("rcp%d" % i, [128, 512], F32) for i in range(2)]
        oCT = [sb("oCT%d" % i, [128, 512], BF16) for i in range(2)]
        hts = [sb("hts%d" % i, [128, D], F32) for i in range(2)]
        junk2 = [sb("junk2%d" % i, [128, D], BF16) for i in range(2)]
        ss2 = [sb("ss2%d" % i, [128, 2], F32) for i in range(2)]
        tmpo = [sb("tmpo%d" % i, [128, D], F32) for i in range(2)]
        ring_of = {}
        rst = {"k": 0, "pt": 0, "t": 0}

        def get_rec(t):
            t = t % NT
            if t in ring_of:
                return ring_of[t]
            slot = ring[rst["k"] % 5]
            rst["k"] += 1
            for k_, v_ in list(ring_of.items()):
                if v_ is slot:
                    del ring_of[k_]
            c.dma("sp", slot, slot.ap[:], REC.ap[t], reads=[REC])
            ring_of[t] = slot
            return slot

        def attention(qrec, kblocks, par):
            oc = oCT[par]
            nkb = len(kblocks)
            steps = [(g, bi, kr, mi) for g in range(2) for bi, (kr, mi) in enumerate(kblocks)]
            slots = []

            def emit_S(i):
                g, bi, kr, mi = steps[i]
                sbk = banks[2 + (rst["pt"] % 2)]
                pT = pTs[rst["pt"] % 3]
                rst["pt"] += 1
                slots.append(pT)
                c.op("pe", lambda e, kr=kr, sbk=sbk, g=g: e.matmul(
                    sbk.ap[:], kr.ap[64 * g:64 * g + 64, 512:640], qrec.ap[64 * g:64 * g + 64, 0:512], start=True, stop=True),
                    reads=[kr, qrec], writes=[sbk], nosync_same=True)
                c.op("act", lambda e, sbk=sbk, pT=pT: e.activation(pT.ap[:], sbk.ap[:], AF.Exp, scale=0.125), reads=[sbk], writes=[pT])
                if mi is not None:
                    c.op("dve", lambda e, pT=pT, mi=mi: e.tensor_tensor(
                        pT.ap[:].rearrange("p (c q) -> p c q", q=128), pT.ap[:].rearrange("p (c q) -> p c q", q=128),
                        masks.ap[:, mi, :].unsqueeze(1).broadcast_to([128, 4, 128]), ALU.mult), reads=[pT, masks], writes=[pT])

            def emit_PV(i):
                g, bi, kr, mi = steps[i]
                pT = slots[i]
                acc = banks[4 + g]
                c.op("pe", lambda e, kr=kr, pT=pT, acc=acc, g=g, bi=bi: e.matmul(
                    acc.ap[:], kr.ap[:, 640 + 64 * g:640 + 64 * g + 128], pT.ap[:], start=(bi == 0), stop=(bi == nkb - 1)),
                    reads=[kr, pT], writes=[acc], nosync_same=True)
                if bi == nkb - 1:
                    pn = slice(0, 64) if g == 0 else slice(64, 128)
                    pd = slice(64, 128) if g == 0 else slice(0, 64)
                    d_, r_ = dn[g], rcp[g]
                    c.op("dve", lambda e, acc=acc, pd=pd, d_=d_: e.tensor_tensor(d_.ap[pd, :], acc.ap[pd, :], esink.ap[pd, :], ALU.add),
                         reads=[acc, esink], writes=[d_])
                    c.op("dve", lambda e, pd=pd, pn=pn, d_=d_, r_=r_: e.reciprocal(r_.ap[pn, :], d_.ap[pd, :]), reads=[d_], writes=[r_])
                    c.op("dve", lambda e, acc=acc, pn=pn, r_=r_, oc=oc: e.tensor_tensor(oc.ap[pn, :], acc.ap[pn, :], r_.ap[pn, :], ALU.mult),
                         reads=[acc, r_], writes=[oc])
            n = len(steps)
            emit_S(0)
            for i in range(1, n):
                emit_S(i)
                emit_PV(i - 1)
            emit_PV(n - 1)
            return oc

        def out_proj(oat_buf, oat_ap, obt_buf, obt_ap_fn, oc, h_src_ap, h_src_reads, who, dst_buf, dst_ap):
            par = rst["t"] % 2
            rst["t"] += 1
            ht, jk, s2, tm = hts[par], junk2[par], ss2[par], tmpo[par]
            c.dma("sp", ht, ht.ap[:], h_src_ap, reads=h_src_reads)
            mb = [banks[6], banks[7]]
            for hh in range(2):
                ops = []
                for j in range(2):
                    ops.append((oat_ap[:, j * 128:(j + 1) * 128], woAB.ap[:, j, hh * 512:(hh + 1) * 512], [oat_buf, woAB]))
                for j in range(2):
                    ops.append((obt_ap_fn(j), woAB.ap[:, 2 + j, hh * 512:(hh + 1) * 512], [obt_buf, woAB]))
                for cc in range(4):
                    ops.append((oc.ap[:, cc * 128:(cc + 1) * 128], woC.ap[:, cc, hh * 512:(hh + 1) * 512], [oc, woC]))
                for oi, (l_, r_, rd) in enumerate(ops):
                    c.op("pe", lambda e, l_=l_, r_=r_, oi=oi, hh=hh: e.matmul(mb[hh].ap[:], l_, r_, start=(oi == 0), stop=(oi == 7)),
                         reads=rd, writes=[mb[hh]], nosync_same=True)
            c.op("pool", lambda e: e.memset(s2.ap[:], 0.0), writes=[s2])
            for hh in range(2):
                c.op("act", lambda e, hh=hh: e.activation(jk.ap[:, hh * 512:(hh + 1) * 512], mb[hh].ap[:], AF.Square, accum_out=s2.ap[:, hh:hh + 1]),
                     reads=[mb[hh]], writes=[jk, s2])
            c.op("dve", lambda e: e.tensor_tensor(s2.ap[:, 0:1], s2.ap[:, 0:1], s2.ap[:, 1:2], ALU.add), reads=[s2], writes=[s2])
            c.op("act", lambda e: e.activation(s2.ap[:, 0:1], s2.ap[:, 0:1], AF.Sqrt, bias=1e-6, scale=1.0 / D), reads=[s2], writes=[s2])
            c.op("dve", lambda e: e.reciprocal(s2.ap[:, 0:1], s2.ap[:, 0:1]), reads=[s2], writes=[s2])
            for hh in range(2):
                c.op("dve", lambda e, hh=hh: e.scalar_tensor_tensor(
                    tm.ap[:, hh * 512:(hh + 1) * 512], mb[hh].ap[:], s2.ap[:, 0:1], G[0][who].ap[:, hh * 512:(hh + 1) * 512], ALU.mult, ALU.mult),
                    reads=[mb[hh], s2, G[0][who]], writes=[tm])
            c.op("pool", lambda e: e.tensor_tensor(tm.ap[:], tm.ap[:], ht.ap[:], ALU.add), reads=[tm, ht], writes=[tm])
            c.dma("pool", dst_buf, dst_ap, tm.ap[:], reads=[tm], writes=[])

        n_own = NT if L == 0 else NH
        KB = 4
        tabv = tab.ap
        nkblk = n_own // KB
        ngrp = NT // TG
        bst = {"g": 0}

        def emit_B(kb, g_lo, g_hi):
            pbs = [banks[0], banks[1]]
            for gi in range(g_lo, g_hi):
                tbf = tbufs[bst["g"] % 4]
                bst["g"] += 1
                c.dma("sp", tbf, tbf.ap[:], tabv[:, gi * TG:(gi + 1) * TG, :, kb * 512:(kb + 1) * 512])
                for nci in range(TG):
                    ncx = gi * TG + nci
                    for part in range(2):
                        for f in range(2):
                            first = (ncx == 0 and part == 0)
                            lastm = (ncx == NT - 1 and part == 1)
                            c.op("pe", lambda e, tbf=tbf, nci=nci, ncx=ncx, part=part, f=f, first=first, lastm=lastm: e.matmul(
                                pbs[f].ap[:], P_all.ap[:, ncx, part * 256 + f * 128:part * 256 + (f + 1) * 128], tbf.ap[:, nci, part, :],
                                start=first, stop=lastm), reads=[P_all, tbf], writes=[pbs[f]], nosync_same=True)
            if g_hi == ngrp:
                ob = oBT[kb % 2]
                for f in range(2):
                    c.op("act", lambda e, f=f, ob=ob: e.copy(ob.ap[:, f, :], pbs[f].ap[:]), reads=[pbs[f]], writes=[ob])

        emit_B(0, 0, ngrp)
        pend2 = None
        for t in range(n_own):
            kb, ti = t // KB, t % KB
            ob = oBT[kb % 2]
            rp, rc_, rn = get_rec(t - 1), get_rec(t), get_rec(t + 1)
            mp = 2 if t == 0 else (3 if t == NH else 0)
            mn = 4 if t == NH - 1 else (5 if t == NT - 1 else 1)
            oc = attention(rc_, [(rp, mp), (rc_, None), (rn, mn), (recC[0], None), (recC[1], None)], t % 2)
            if kb + 1 < nkblk:
                emit_B(kb + 1, ti * ngrp // KB, (ti + 1) * ngrp // KB)
            if pend2 is not None:
                pend2()
            oa = oats[t % 2]
            c.dma("sp", oa, oa.ap[:], OAT.ap[t], reads=[OAT])
            src = xb.ap[t * 128:(t + 1) * 128, :] if L == 0 else H1.ap[t]

            def mk(oa=oa, ob=ob, ti=ti, oc=oc, src=src, t=t):
                return lambda: out_proj(oa, oa.ap, ob, lambda j: ob.ap[:, j, ti * 128:(ti + 1) * 128], oc, src, ([] if L == 0 else [H1]), 0, HM, HM.ap[t])
            pend2 = mk()
        pend2()
        if not last:
            obc = oBT[0]
            pbk = banks[0]
            for f in range(2):
                k_ = 0
                for ncx in range(2):
                    for part in range(2):
                        c.op("pe", lambda e, f=f, ncx=ncx, part=part, k_=k_: e.matmul(
                            pbk.ap[:, f * 256:(f + 1) * 256], P_ctx.ap[:, ncx, part * 256 + f * 128:part * 256 + (f + 1) * 128],
                            tabc_sb.ap[:, ncx, part, :], start=(k_ == 0), stop=(k_ == 3)), reads=[P_ctx, tabc_sb], writes=[pbk], nosync_same=True)
                        k_ += 1
            c.op("act", lambda e: e.copy(obc.ap[:, :, 0:256], pbk.ap[:].rearrange("p (f k) -> p f k", f=2)), reads=[pbk], writes=[obc])
            for i in range(2):
                oc = attention(recC[i], [(recC[0], None), (recC[1], None)], i)
                out_proj(oatC, oatC.ap[:, i, :], obc, lambda j, i=i: obc.ap[:, j, i * 128:(i + 1) * 128], oc,
                         ctxb.ap[i * 128:(i + 1) * 128, :], [], 1, HCM, HCM.ap[i])
        c.barrier()
        chk("pass2_%d" % L)
        st["off"] = mixer_top

        moe = (L == 1)
        nbf = [norm_bufs("p3%d" % i) for i in range(2)]
        hm4 = [sb("hm4%d" % i, [128, 4, D], F32) for i in range(1)]
        y2T = [sb("y2T%d" % i, [128, 8, 512], BF16) for i in range(1)]
        actTs = [sb("actT%d" % i, [128, FG, 512], BF16) for i in range(3)]
        wgb = [sb("wgb%d" % i, [128, 8, FG * 128], BF16) for i in range(3)]
        wub = [sb("wub%d" % i, [128, 8, FG * 128], BF16) for i in range(3)]
        wdb = [sb("wdb%d" % i, [128, FG, D], BF16) for i in range(3)]
        sg = [sb("sg%d" % i, [128, 512], F32) for i in range(2)]
        ss3 = [sb("ss3%d" % i, [128, 2], F32) for i in range(2)]
        junk3 = [sb("junk3%d" % i, [128, D], BF16) for i in range(2)]
        tm3 = [sb("tm3%d" % i, [128, D], F32) for i in range(2)]
        wst = {"g": 0, "gu": 0, "f": 0, "blk": 0}
        if moe and not SPARSE_MOE:
            acc4 = sb("acc4", [128, 4, D], F32)
            wr_sb = sb("wr_sb", [128, 8, NE], BF16)
            c.dma("pool", wr_sb, wr_sb.ap[:], w_r.ap.rearrange("(c p) n -> p c n", p=128))
            brs = sb("brs", [128, NE], F32)
            c.dma("sp", brs, brs.ap[:], b_rb.ap[:])
            lg = [sb("lg%d" % i, [128, NE], F32) for i in range(4)]
            comb = [sb("comb%d" % i, [128, NE], F32) for i in range(4)]
            rt = dict(m1=sb("rt_m1", [128, 1], F32), m2=sb("rt_m2", [128, 1], F32), k1=sb("rt_k1", [128, NE], F32),
                      k2=sb("rt_k2", [128, NE], F32), l2=sb("rt_l2", [128, NE], F32), g1=sb("rt_g1", [128, 1], F32), g2=sb("rt_g2", [128, 1], F32))

        def ffn_block(tiles_src, nt, who, dsts, wsets):
            bp = 0
            wst["blk"] += 1
            hm, yT = hm4[bp], y2T[bp]
            ntok = nt * 128
            for i, (sap, rd) in enumerate(tiles_src):
                nb = nbf[i % 2]
                c.dma("sp", nb["ht"], nb["ht"].ap[:], sap, reads=rd)
                c.op("pool", lambda e, nb=nb, i=i: e.tensor_copy(hm.ap[:, i, :], nb["ht"].ap[:]), reads=[nb["ht"]], writes=[hm])
                emit_norm_T(nb, who, 1, lambda ci, i=i: yT.ap[:, ci, i * 128:(i + 1) * 128], yT, banks[0])
                if moe:
                    lb = banks[1]
                    for ci in range(8):
                        c.op("pe", lambda e, ci=ci, i=i: e.matmul(lb.ap[:, 0:NE], yT.ap[:, ci, i * 128:(i + 1) * 128], wr_sb.ap[:, ci, :], start=(ci == 0), stop=(ci == 7)),
                             reads=[yT, wr_sb], writes=[lb], nosync_same=True)
                    l_, cb = lg[i], comb[i]
                    c.op("dve", lambda e, l_=l_: e.tensor_tensor(l_.ap[:], lb.ap[:, 0:NE], brs.ap[:], ALU.add), reads=[lb, brs], writes=[l_])
                    c.op("dve", lambda e, l_=l_: e.tensor_reduce(rt["m1"].ap[:], l_.ap[:], AX.X, ALU.max), reads=[l_], writes=[rt["m1"]])
                    c.op("dve", lambda e, l_=l_: e.tensor_scalar(rt["k1"].ap[:], l_.ap[:], rt["m1"].ap[:, 0:1], None, ALU.is_equal), reads=[l_, rt["m1"]], writes=[rt["k1"]])
                    c.op("dve", lambda e, l_=l_: e.scalar_tensor_tensor(rt["l2"].ap[:], rt["k1"].ap[:], -1e30, l_.ap[:], ALU.mult, ALU.add), reads=[l_, rt["k1"]], writes=[rt["l2"]])
                    c.op("dve", lambda e: e.tensor_reduce(rt["m2"].ap[:], rt["l2"].ap[:], AX.X, ALU.max), reads=[rt["l2"]], writes=[rt["m2"]])
                    c.op("dve", lambda e: e.tensor_scalar(rt["k2"].ap[:], rt["l2"].ap[:], rt["m2"].ap[:, 0:1], None, ALU.is_equal), reads=[rt["l2"], rt["m2"]], writes=[rt["k2"]])
                    c.op("dve", lambda e: e.tensor_tensor(rt["g2"].ap[:], rt["m2"].ap[:], rt["m1"].ap[:], ALU.subtract), reads=[rt["m1"], rt["m2"]], writes=[rt["g2"]])
                    c.op("act", lambda e: e.activation(rt["g2"].ap[:], rt["g2"].ap[:], AF.Sigmoid), reads=[rt["g2"]], writes=[rt["g2"]])
                    c.op("dve", lambda e: e.tensor_scalar(rt["g1"].ap[:], rt["g2"].ap[:], -1.0, 1.0, ALU.mult, ALU.add), reads=[rt["g2"]], writes=[rt["g1"]])
                    c.op("dve", lambda e, cb=cb: e.tensor_scalar(cb.ap[:], rt["k1"].ap[:], rt["g1"].ap[:, 0:1], None, ALU.mult), reads=[rt["k1"], rt["g1"]], writes=[cb])
                    c.op("dve", lambda e, cb=cb: e.scalar_tensor_tensor(cb.ap[:], rt["k2"].ap[:], rt["g2"].ap[:, 0:1], cb.ap[:], ALU.mult, ALU.add), reads=[rt["k2"], rt["g2"], cb], writes=[cb])
            pend_down = [None]
            for ei, (wga, wua, wda) in enumerate(wsets):
                for gi in range(NG):
                    wp = wst["g"] % 3
                    wst["g"] += 1
                    wg_, wu_, wd_ = wgb[wp], wub[wp], wdb[wp]
                    actT = actTs[wp]
                    cols = slice(gi * FG * 128, (gi + 1) * FG * 128)
                    if wga is None:
                        c.dma("sp", wg_, wg_.ap[:], WGB.ap[gi])
                        c.dma("sp", wu_, wu_.ap[:], WUB.ap[gi])
                        c.dma("sp", wd_, wd_.ap[:], WDB.ap[gi])
                    else:
                        c.dma("pool", wg_, wg_.ap[:], wga.rearrange("(c p) n -> p c n", p=128)[:, :, cols])
                        c.dma("pool", wu_, wu_.ap[:], wua.rearrange("(c p) n -> p c n", p=128)[:, :, cols])
                        c.dma("pool", wd_, wd_.ap[:], wda[gi * FG * 128:(gi + 1) * FG * 128, :].rearrange("(c p) n -> p c n", p=128))
                    for fj in range(FG):
                        j = gi * FG + fj
                        gp_ = wst["gu"] % 2
                        wst["gu"] += 1
                        gb, ub = banks[gp_ * 2], banks[gp_ * 2 + 1]
                        for ci in range(8):
                            c.op("pe", lambda e, ci=ci, fj=fj, wg_=wg_, gb=gb: e.matmul(
                                gb.ap[:, 0:ntok], wg_.ap[:, ci, fj * 128:(fj + 1) * 128], yT.ap[:, ci, 0:ntok], start=(ci == 0), stop=(ci == 7)),
                                reads=[wg_, yT], writes=[gb], nosync_same=True)
                        for ci in range(8):
                            c.op("pe", lambda e, ci=ci, fj=fj, wu_=wu_, ub=ub: e.matmul(
                                ub.ap[:, 0:ntok], wu_.ap[:, ci, fj * 128:(fj + 1) * 128], yT.ap[:, ci, 0:ntok], start=(ci == 0), stop=(ci == 7)),
                                reads=[wu_, yT], writes=[ub], nosync_same=True)
                        s_ = sg[gp_]
                        c.op("act", lambda e, gb=gb, s_=s_: e.activation(s_.ap[:, 0:ntok], gb.ap[:, 0:ntok], AF.Silu), reads=[gb], writes=[s_])
                        c.op("dve", lambda e, ub=ub, s_=s_, fj=fj, actT=actT: e.tensor_tensor(actT.ap[:, fj, 0:ntok], ub.ap[:, 0:ntok], s_.ap[:, 0:ntok], ALU.mult),
                             reads=[ub, s_], writes=[actT])
                    def down(gi=gi, wd_=wd_, actT=actT):
                        for i in range(nt):
                            fp_ = wst["f"] % 2
                            wst["f"] += 1
                            fb_ = [banks[4 + fp_ * 2], banks[5 + fp_ * 2]]
                            for hh in range(2):
                                for fj in range(FG):
                                    c.op("pe", lambda e, i=i, hh=hh, fj=fj, wd_=wd_, fb_=fb_, actT=actT: e.matmul(
                                        fb_[hh].ap[:], actT.ap[:, fj, i * 128:(i + 1) * 128], wd_.ap[:, fj, hh * 512:(hh + 1) * 512],
                                        start=(fj == 0), stop=(fj == FG - 1)), reads=[actT, wd_], writes=[fb_[hh]], nosync_same=True)
                            for hh in range(2):
                                dst = facc.ap[:, i, hh * 512:(hh + 1) * 512]
                                if gi == 0:
                                    c.op("act", lambda e, dst=dst, hh=hh, fb_=fb_: e.copy(dst, fb_[hh].ap[:]), reads=[fb_[hh]], writes=[facc])
                                else:
                                    c.op("dve", lambda e, dst=dst, hh=hh, fb_=fb_: e.tensor_tensor(dst, fb_[hh].ap[:], dst, ALU.add), reads=[fb_[hh], facc], writes=[facc])
                    if pend_down[0] is not None:
                        pend_down[0]()
                    pend_down[0] = down
                pend_down[0]()
                pend_down[0] = None
                if moe:
                    for i in range(nt):
                        for hh in range(2):
                            dst = acc4.ap[:, i, hh * 512:(hh + 1) * 512]
                            srcf = facc.ap[:, i, hh * 512:(hh + 1) * 512]
                            if ei == 0:
                                c.op("dve", lambda e, dst=dst, srcf=srcf, i=i, ei=ei: e.tensor_scalar(dst, srcf, comb[i].ap[:, ei:ei + 1], None, ALU.mult),
                                     reads=[facc, comb[i]], writes=[acc4])
                            else:
                                c.op("dve", lambda e, dst=dst, srcf=srcf, i=i, ei=ei: e.scalar_tensor_tensor(dst, srcf, comb[i].ap[:, ei:ei + 1], dst, ALU.mult, ALU.add),
                                     reads=[facc, comb[i], acc4], writes=[acc4])
            fin = acc4 if moe else facc
            for i in range(nt):
                p_ = i % 2
                s3, jk, tm = ss3[p_], junk3[p_], tm3[p_]
                c.op("pool", lambda e, s3=s3: e.memset(s3.ap[:], 0.0), writes=[s3])
                c.op("act", lambda e, i=i, s3=s3, jk=jk: e.activation(jk.ap[:], fin.ap[:, i, :], AF.Square, accum_out=s3.ap[:, 0:1]), reads=[fin], writes=[jk, s3])
                c.op("act", lambda e, s3=s3: e.activation(s3.ap[:, 0:1], s3.ap[:, 0:1], AF.Sqrt, bias=1e-6, scale=1.0 / D), reads=[s3], writes=[s3])
                c.op("dve", lambda e, s3=s3: e.reciprocal(s3.ap[:, 0:1], s3.ap[:, 0:1]), reads=[s3], writes=[s3])
                c.op("dve", lambda e, i=i, s3=s3, tm=tm: e.scalar_tensor_tensor(tm.ap[:], fin.ap[:, i, :], s3.ap[:, 0:1], G[1][who].ap[:], ALU.mult, ALU.mult),
                     reads=[fin, s3, G[1][who]], writes=[tm])
                c.op("pool", lambda e, i=i, tm=tm: e.tensor_tensor(tm.ap[:], tm.ap[:], hm.ap[:, i, :], ALU.add), reads=[tm, hm], writes=[tm])
                dbuf, dap = dsts[i]
                c.dma("sp", dbuf, dap, tm.ap[:], reads=[tm], writes=[])


        def idma(gather, out_ap, in_ap, idx_ap, reads, writes):
            def fn(e):
                if gather:
                    return e.indirect_dma_start(out=out_ap, out_offset=None, in_=in_ap,
                                                in_offset=bass.IndirectOffsetOnAxis(ap=idx_ap, axis=0))
                return e.indirect_dma_start(out=out_ap, out_offset=bass.IndirectOffsetOnAxis(ap=idx_ap, axis=0),
                                            in_=in_ap, in_offset=None)
            c.dma_custom("pool", fn, reads, writes)

        def moe_sparse():
            st["off"] = mixer_top
            base = st["off"]
            K1 = sb("K1", [128, NHt, NE], F32)
            K2 = sb("K2", [128, NHt, NE], F32)
            POS = sb("POS", [128, NHt, NE], F32)
            G1 = sb("G1", [128, NHt], F32)
            G2 = sb("G2", [128, NHt], F32)
            P1f = sb("P1f", [128, NHt], F32)
            P2f = sb("P2f", [128, NHt], F32)
            IDX1 = sb("IDX1", [128, NHt], I32)
            IDX2 = sb("IDX2", [128, NHt], I32)
            carry = sb("carry", [128, NE], F32)
            offv = sb("offv", [128, NE], F32)
            endv = sb("endv", [128, NE], F32)
            npf = sb("npf", [128, NE], F32)
            Es = sb("Es", [128, NS], F32)
            EsW = sb("EsW", [128, NS], F32)
            EsD = sb("EsD", [128, NS], F32)
            ltri = sb("ltri", [128, 128], BF16)
            onesb = sb("onesb", [128, 128], BF16)
            cW = sb("cW", [128, 8 * NG], F32)
            cD = sb("cD", [128, NFC], F32)
            wr_sb = sb("wr_sb", [128, 8, NE], BF16)
            brs = sb("brs", [128, NE], F32)
            c.dma("sp", ltri, ltri.ap[:], ltrid.ap[:])
            c.dma("sp", cW, cW.ap[:], constWd.ap[:])
            c.dma("sp", cD, cD.ap[:], constDd.ap[:])
            c.dma("pool", wr_sb, wr_sb.ap[:], w_r.ap.rearrange("(c p) n -> p c n", p=128))
            c.dma("sp", brs, brs.ap[:], b_rb.ap[:])
            c.op("dve", lambda e: e.memset(onesb.ap[:], 1.0), writes=[onesb])
            c.op("dve", lambda e: e.memset(carry.ap[:], 0.0), writes=[carry])
            state_end = st["off"]

            nbr = [norm_bufs("r1%d" % i) for i in range(2)]
            yTr = [sb("yTr%d" % i, [128, 8, 128], BF16) for i in range(2)]
            tmpf = [sb("tmpf%d" % i, [128, D], F32) for i in range(2)]
            y2t = [sb("y2t%d" % i, [128, D], BF16) for i in range(2)]
            lgt = sb("lgt", [128, NE], F32)
            l2t = sb("l2t", [128, NE], F32)
            m1 = sb("m1", [128, 1], F32)
            m2 = sb("m2", [128, 1], F32)
            Mf = sb("Mf", [128, NE], F32)
            Mb = [sb("Mb%d" % i, [128, NE], BF16) for i in range(2)]
            for i in range(NHt):
                p = i % 2
                nb, yT = nbr[p], yTr[p]
                c.dma("sp", nb["ht"], nb["ht"].ap[:], HM.ap[i], reads=[HM])
                emit_norm_T(nb, 0, 1, lambda ci, yT=yT: yT.ap[:, ci, :], yT, banks[0])
                tf, yt = tmpf[p], y2t[p]
                c.op("dve", lambda e, nb=nb, tf=tf: e.tensor_tensor(tf.ap[:], nb["xn"].ap[:], MULbc.ap[:], ALU.mult), reads=[nb["xn"], MULbc], writes=[tf])
                c.op("pool", lambda e, tf=tf, yt=yt: e.tensor_tensor(yt.ap[:], tf.ap[:], ADDbc.ap[:], ALU.add), reads=[tf, ADDbc], writes=[yt])
                c.dma("pool", Y2, Y2.ap[i], yt.ap[:], reads=[yt], writes=[])
                lb = banks[1]
                for ci in range(8):
                    c.op("pe", lambda e, ci=ci, yT=yT: e.matmul(lb.ap[:, 0:NE], yT.ap[:, ci, :], wr_sb.ap[:, ci, :], start=(ci == 0), stop=(ci == 7)),
                         reads=[yT, wr_sb], writes=[lb], nosync_same=True)
                k1, k2 = K1.ap[:, i, :], K2.ap[:, i, :]
                g1, g2 = G1.ap[:, i:i + 1], G2.ap[:, i:i + 1]
                c.op("dve", lambda e: e.tensor_tensor(lgt.ap[:], lb.ap[:, 0:NE], brs.ap[:], ALU.add), reads=[lb, brs], writes=[lgt])
                c.op("dve", lambda e: e.tensor_reduce(m1.ap[:], lgt.ap[:], AX.X, ALU.max), reads=[lgt], writes=[m1])
                c.op("dve", lambda e, k1=k1: e.tensor_scalar(k1, lgt.ap[:], m1.ap[:, 0:1], None, ALU.is_equal), reads=[lgt, m1], writes=[K1])
                c.op("dve", lambda e, k1=k1: e.scalar_tensor_tensor(l2t.ap[:], k1, -1e30, lgt.ap[:], ALU.mult, ALU.add), reads=[lgt, K1], writes=[l2t])
                c.op("dve", lambda e: e.tensor_reduce(m2.ap[:], l2t.ap[:], AX.X, ALU.max), reads=[l2t], writes=[m2])
                c.op("dve", lambda e, k2=k2: e.tensor_scalar(k2, l2t.ap[:], m2.ap[:, 0:1], None, ALU.is_equal), reads=[l2t, m2], writes=[K2])
                c.op("dve", lambda e, g2=g2: e.tensor_tensor(g2, m2.ap[:], m1.ap[:], ALU.subtract), reads=[m1, m2], writes=[G2])
                c.op("act", lambda e, g2=g2: e.activation(g2, g2, AF.Sigmoid), reads=[G2], writes=[G2])
                c.op("dve", lambda e, g1=g1, g2=g2: e.tensor_scalar(g1, g2, -1.0, 1.0, ALU.mult, ALU.add), reads=[G2], writes=[G1])
                mb_ = Mb[p]
                c.op("dve", lambda e, k1=k1, k2=k2, mb_=mb_: e.tensor_tensor(mb_.ap[:], k1, k2, ALU.add), reads=[K1, K2], writes=[mb_])
                pb_ = banks[2]
                c.op("pe", lambda e, mb_=mb_: e.matmul(pb_.ap[:, 0:NE], ltri.ap[:], mb_.ap[:], start=True, stop=True), reads=[ltri, mb_], writes=[pb_], nosync_same=True)
                c.op("pe", lambda e, mb_=mb_: e.matmul(pb_.ap[:, NE:2 * NE], onesb.ap[:], mb_.ap[:], start=True, stop=True), reads=[onesb, mb_], writes=[pb_], nosync_same=True)
                c.op("dve", lambda e, i=i: e.tensor_tensor(POS.ap[:, i, :], pb_.ap[:, 0:NE], carry.ap[:], ALU.add), reads=[pb_, carry], writes=[POS])
                c.op("dve", lambda e: e.tensor_tensor(carry.ap[:], pb_.ap[:, NE:2 * NE], carry.ap[:], ALU.add), reads=[pb_, carry], writes=[carry])
            c.barrier()
            tq = sb("tq", [128, NE], F32)
            c.op("dve", lambda e: e.memset(npf.ap[:], 0.0), writes=[npf])
            for j in range(T_OWN // 512):
                c.op("dve", lambda e, j=j: e.tensor_scalar(tq.ap[:], carry.ap[:], float(512 * j), None, ALU.is_gt), reads=[carry], writes=[tq])
                c.op("dve", lambda e: e.tensor_tensor(npf.ap[:], npf.ap[:], tq.ap[:], ALU.add), reads=[npf, tq], writes=[npf])
            c.op("dve", lambda e: e.tensor_scalar(npf.ap[:], npf.ap[:], 512.0, None, ALU.mult), reads=[npf], writes=[npf])
            c.op("dve", lambda e: e.memset(offv.ap[:], 0.0), writes=[offv])
            for e_ in range(1, NE):
                c.op("dve", lambda e, e_=e_: e.tensor_tensor(offv.ap[:, e_:e_ + 1], offv.ap[:, e_ - 1:e_], npf.ap[:, e_ - 1:e_], ALU.add),
                     reads=[offv, npf], writes=[offv])
            c.op("dve", lambda e: e.tensor_tensor(endv.ap[:], offv.ap[:], npf.ap[:], ALU.add), reads=[offv, npf], writes=[endv])
            for i in range(NHt):
                for Kx, Px in ((K1, P1f), (K2, P2f)):
                    c.op("dve", lambda e, i=i: e.tensor_tensor(tq.ap[:], POS.ap[:, i, :], offv.ap[:], ALU.add), reads=[POS, offv], writes=[tq])
                    c.op("dve", lambda e, i=i, Kx=Kx: e.tensor_tensor(tq.ap[:], tq.ap[:], Kx.ap[:, i, :], ALU.mult), reads=[tq, Kx], writes=[tq])
                    c.op("dve", lambda e, i=i, Px=Px: e.tensor_reduce(Px.ap[:, i:i + 1], tq.ap[:], AX.X, ALU.add), reads=[tq], writes=[Px])
            c.op("dve", lambda e: e.tensor_copy(IDX1.ap[:], P1f.ap[:]), reads=[P1f], writes=[IDX1])
            c.op("dve", lambda e: e.tensor_copy(IDX2.ap[:], P2f.ap[:]), reads=[P2f], writes=[IDX2])
            for s_ in range(NS):
                c.op("dve", lambda e, s_=s_: e.tensor_scalar(tq.ap[:], endv.ap[:], float(512 * s_), None, ALU.is_le), reads=[endv], writes=[tq])
                c.op("dve", lambda e, s_=s_: e.tensor_reduce(Es.ap[:, s_:s_ + 1], tq.ap[:], AX.X, ALU.add), reads=[tq], writes=[Es])
            c.op("dve", lambda e: e.tensor_scalar(Es.ap[:], Es.ap[:], float(NE - 1), None, ALU.min), reads=[Es], writes=[Es])
            c.op("dve", lambda e: e.tensor_scalar(EsW.ap[:], Es.ap[:], float(1024 * RPD), None, ALU.mult), reads=[Es], writes=[EsW])
            c.op("dve", lambda e: e.tensor_scalar(EsD.ap[:], Es.ap[:], float(DFF), None, ALU.mult), reads=[Es], writes=[EsD])
            for i in range(NHt):
                yt = y2t[i % 2]
                c.dma("sp", yt, yt.ap[:], Y2.ap[i], reads=[Y2])
                idma(False, Y2S.ap[:, :], yt.ap[:], IDX1.ap[:, i:i + 1], [yt, IDX1], [])
                idma(False, Y2S.ap[:, :], yt.ap[:], IDX2.ap[:, i:i + 1], [yt, IDX2], [])
            c.barrier()
            st["off"] = state_end
            ytm = [sb("ytm%d" % i, [128, D], BF16) for i in range(2)]
            yTs = [sb("yTs%d" % i, [128, 8, 512], BF16) for i in range(2)]
            idxWf = sb("idxWf", [128, 8 * NG], F32)
            idxDf = sb("idxDf", [128, NFC], F32)
            idxW = [sb("idxW%d" % i, [128, 8 * NG], I32) for i in range(2)]
            idxD = [sb("idxD%d" % i, [128, NFC], I32) for i in range(2)]
            NWS = 3
            wgs = [sb("wgs%d" % i, [128, 8, 512], BF16) for i in range(NWS)]
            wus = [sb("wus%d" % i, [128, 8, 512], BF16) for i in range(NWS)]
            wds = [sb("wds%d" % i, [128, FG, D], BF16) for i in range(NWS)]
            acts = [sb("acts%d" % i, [128, FG, 512], BF16) for i in range(NWS)]
            sgs = [sb("sgs%d" % i, [128, 512], F32) for i in range(2)]
            faccs = [sb("faccs%d" % i, [128, 4, D], F32) for i in range(2)]
            wgc = [[Buf("wgc", w_.ap[:, ci, :]) for ci in range(8)] for w_ in wgs]
            wuc = [[Buf("wuc", w_.ap[:, ci, :]) for ci in range(8)] for w_ in wus]
            wdc = [[Buf("wdc", w_.ap[:, fj, :]) for fj in range(FG)] for w_ in wds]
            wgf = wg_e.ap.rearrange("e d (r n) -> (e d r) n", n=512)
            wuf = wu_e.ap.rearrange("e d (r n) -> (e d r) n", n=512)
            wdf = wd_e.ap.rearrange("e f n -> (e f) n")
            sst = {"g": 0, "gu": 0, "f": 0}
            spend = [None]
            for s_ in range(NS):
                sp_ = s_ % 2
                iw, idd, yT, fa = idxW[sp_], idxD[sp_], yTs[sp_], faccs[sp_]
                c.op("dve", lambda e, s_=s_: e.tensor_scalar(idxWf.ap[:], cW.ap[:], EsW.ap[:, s_:s_ + 1], None, ALU.add), reads=[cW, EsW], writes=[idxWf])
                c.op("dve", lambda e, iw=iw: e.tensor_copy(iw.ap[:], idxWf.ap[:]), reads=[idxWf], writes=[iw])
                c.op("dve", lambda e, s_=s_: e.tensor_scalar(idxDf.ap[:], cD.ap[:], EsD.ap[:, s_:s_ + 1], None, ALU.add), reads=[cD, EsD], writes=[idxDf])
                c.op("dve", lambda e, idd=idd: e.tensor_copy(idd.ap[:], idxDf.ap[:]), reads=[idxDf], writes=[idd])
                for i in range(4):
                    ym = ytm[i % 2]
                    c.dma("sp", ym, ym.ap[:], Y2S.ap[s_ * 512 + i * 128:s_ * 512 + (i + 1) * 128, :], reads=[Y2S])
                    tb = banks[0]
                    tvv = bfv(tb)
                    for ci in range(8):
                        c.op("pe", lambda e, ci=ci, ym=ym: e.transpose(tvv[:, ci * 128:(ci + 1) * 128], ym.ap[:, ci * 128:(ci + 1) * 128], ident_b.ap[:]),
                             reads=[ym, ident_b], writes=[tb], nosync_same=True)
                    c.op("act", lambda e, i=i, yT=yT: e.copy(yT.ap[:, :, i * 128:(i + 1) * 128], tvv.rearrange("p (c t) -> p c t", t=128)),
                         reads=[tb], writes=[yT])
                for gi in range(NG):
                    wp = sst["g"] % NWS
                    sst["g"] += 1
                    wg_, wu_, wd_, actT = wgs[wp], wus[wp], wds[wp], acts[wp]
                    for ci in range(8):
                        idma(True, wg_.ap[:, ci, :], wgf, iw.ap[:, ci * NG + gi:ci * NG + gi + 1], [iw], [wgc[wp][ci]])
                    for ci in range(8):
                        idma(True, wu_.ap[:, ci, :], wuf, iw.ap[:, ci * NG + gi:ci * NG + gi + 1], [iw], [wuc[wp][ci]])
                    for fj in range(FG):
                        k_ = gi * FG + fj
                        idma(True, wd_.ap[:, fj, :], wdf, idd.ap[:, k_:k_ + 1], [idd], [wdc[wp][fj]])
                    for fj in range(FG):
                        gp_ = sst["gu"] % 2
                        sst["gu"] += 1
                        gb, ub = banks[gp_ * 2], banks[gp_ * 2 + 1]
                        for ci in range(8):
                            c.op("pe", lambda e, ci=ci, fj=fj, wg_=wg_, gb=gb, yT=yT: e.matmul(
                                gb.ap[:], wg_.ap[:, ci, fj * 128:(fj + 1) * 128], yT.ap[:, ci, :], start=(ci == 0), stop=(ci == 7)),
                                reads=[wgc[wp][ci], yT], writes=[gb], nosync_same=True)
                        for ci in range(8):
                            c.op("pe", lambda e, ci=ci, fj=fj, wu_=wu_, ub=ub, yT=yT: e.matmul(
                                ub.ap[:], wu_.ap[:, ci, fj * 128:(fj + 1) * 128], yT.ap[:, ci, :], start=(ci == 0), stop=(ci == 7)),
                                reads=[wuc[wp][ci], yT], writes=[ub], nosync_same=True)
                        sg_ = sgs[gp_]
                        c.op("act", lambda e, gb=gb, sg_=sg_: e.activation(sg_.ap[:], gb.ap[:], AF.Silu), reads=[gb], writes=[sg_])
                        c.op("dve", lambda e, ub=ub, sg_=sg_, fj=fj, actT=actT: e.tensor_tensor(actT.ap[:, fj, :], ub.ap[:], sg_.ap[:], ALU.mult),
                             reads=[ub, sg_], writes=[actT])
                    def sdown(gi=gi, wp=wp, wd_=wd_, actT=actT, fa=fa):
                        for i in range(4):
                            fp_ = sst["f"] % 2
                            sst["f"] += 1
                            fb_ = [banks[4 + fp_ * 2], banks[5 + fp_ * 2]]
                            for hh in range(2):
                                for fj in range(FG):
                                    c.op("pe", lambda e, i=i, hh=hh, fj=fj, wd_=wd_, fb_=fb_, actT=actT: e.matmul(
                                        fb_[hh].ap[:], actT.ap[:, fj, i * 128:(i + 1) * 128], wd_.ap[:, fj, hh * 512:(hh + 1) * 512],
                                        start=(fj == 0), stop=(fj == FG - 1)), reads=[actT, wdc[wp][fj]], writes=[fb_[hh]], nosync_same=True)
                            for hh in range(2):
                                dst = fa.ap[:, i, hh * 512:(hh + 1) * 512]
                                if gi == 0:
                                    c.op("act", lambda e, dst=dst, hh=hh, fb_=fb_: e.copy(dst, fb_[hh].ap[:]), reads=[fb_[hh]], writes=[fa])
                                else:
                                    c.op("dve", lambda e, dst=dst, hh=hh, fb_=fb_: e.tensor_tensor(dst, fb_[hh].ap[:], dst, ALU.add), reads=[fb_[hh], fa], writes=[fa])
                    if spend[0] is not None:
                        spend[0]()
                    spend[0] = sdown
                spend[0]()
                spend[0] = None
                for i in range(4):
                    c.dma("sp", RS, RS.ap[s_ * 512 + i * 128:s_ * 512 + (i + 1) * 128, :], fa.ap[:, i, :], reads=[fa], writes=[])
            c.barrier()
            st["off"] = state_end
            r1 = [sb("r1t%d" % i, [128, D], F32) for i in range(2)]
            r2 = [sb("r2t%d" % i, [128, D], F32) for i in range(2)]
            hmt = [sb("hmt%d" % i, [128, D], F32) for i in range(2)]
            jk3 = [sb("jk3%d" % i, [128, D], BF16) for i in range(2)]
            s3s = [sb("s3s%d" % i, [128, 2], F32) for i in range(2)]
            for i in range(NHt):
                p = i % 2
                a, b, hm_, jk, s3 = r1[p], r2[p], hmt[p], jk3[p], s3s[p]
                idma(True, a.ap[:], RS.ap[:, :], IDX1.ap[:, i:i + 1], [IDX1, RS], [a])
                idma(True, b.ap[:], RS.ap[:, :], IDX2.ap[:, i:i + 1], [IDX2, RS], [b])
                c.dma("sp", hm_, hm_.ap[:], HM.ap[i], reads=[HM])
                c.op("dve", lambda e, a=a, i=i: e.tensor_scalar(a.ap[:], a.ap[:], G1.ap[:, i:i + 1], None, ALU.mult), reads=[a, G1], writes=[a])
                c.op("dve", lambda e, a=a, b=b, i=i: e.scalar_tensor_tensor(a.ap[:], b.ap[:], G2.ap[:, i:i + 1], a.ap[:], ALU.mult, ALU.add), reads=[a, b, G2], writes=[a])
                c.op("dve", lambda e, s3=s3: e.memset(s3.ap[:], 0.0), writes=[s3])
                c.op("act", lambda e, a=a, jk=jk, s3=s3: e.activation(jk.ap[:], a.ap[:], AF.Square, accum_out=s3.ap[:, 0:1]), reads=[a], writes=[jk, s3])
                c.op("act", lambda e, s3=s3: e.activation(s3.ap[:, 0:1], s3.ap[:, 0:1], AF.Sqrt, bias=1e-6, scale=1.0 / D), reads=[s3], writes=[s3])
                c.op("dve", lambda e, s3=s3: e.reciprocal(s3.ap[:, 0:1], s3.ap[:, 0:1]), reads=[s3], writes=[s3])
                c.op("dve", lambda e, a=a, b=b, s3=s3: e.scalar_tensor_tensor(b.ap[:], a.ap[:], s3.ap[:, 0:1], G[1][0].ap[:], ALU.mult, ALU.mult),
                     reads=[a, s3, G[1][0]], writes=[b])
                c.op("dve", lambda e, b=b, hm_=hm_: e.tensor_tensor(b.ap[:], b.ap[:], hm_.ap[:], ALU.add), reads=[b, hm_], writes=[b])
                c.dma("sp", out, out.ap[i * 128:(i + 1) * 128, :], b.ap[:], reads=[b], writes=[])
            st["off"] = base

        facc = sb("facc", [128, 4, D], F32)
        if moe and SPARSE_MOE:
            moe_sparse()
        elif not moe:
            wset = [(None, None, None)] if PRECONV else [(wg_d.ap, wu_d.ap, wd_d.ap)]
            for b_ in range(NT // 4):
                ts = [(HM.ap[b_ * 4 + i], [HM]) for i in range(4)]
                ds = [(H1, H1.ap[b_ * 4 + i]) for i in range(4)]
                ffn_block(ts, 4, 0, ds, wset)
            ts = [(HCM.ap[i], [HCM]) for i in range(2)]
            ds = [(HC1, HC1.ap[i]) for i in range(2)]
            ffn_block(ts, 2, 1, ds, wset)
        else:
            wset = [(wg_e.ap[e_], wu_e.ap[e_], wd_e.ap[e_]) for e_ in range(NE)]
            for b_ in range(NH // 4):
                ts = [(HM.ap[b_ * 4 + i], [HM]) for i in range(4)]
                ds = [(out, out.ap[(b_ * 4 + i) * 128:(b_ * 4 + i + 1) * 128, :]) for i in range(4)]
                ffn_block(ts, 4, 0, ds, wset)
        c.barrier()

    try:
        chk("init")
        layer(0)
        chk("layer0")
        layer(1)
    except _Stop:
        pass
    c.finish()
    return nc


_CONST_CACHE = {}


def _consts(cfg, s):
    key = (cfg.SEQ, s)
    if key in _CONST_CACHE:
        return _CONST_CACHE[key]
    SEQ, NT, NH = cfg.SEQ, cfg.NT, cfg.NH
    bf = ml_dtypes.bfloat16
    g = (np.arange(SEQ, dtype=np.int64) + s * (SEQ // 2)) % SEQ
    inv = (10000.0 ** (-(np.arange(0, 32, 2, dtype=np.float32) / 32.0))).astype(np.float32)
    row = (g // 64).astype(np.float32)
    col = (g % 64).astype(np.float32)
    ang = np.concatenate([row[:, None] * inv[None, :], col[:, None] * inv[None, :]], axis=1).astype(np.float32)
    rc = np.ones((NT + 1, 128, 32), np.float32)
    rs = np.zeros((NT + 1, 128, 32), np.float32)
    rc[:NT] = np.cos(ang).reshape(NT, 128, 32)
    rs[:NT] = np.sin(ang).reshape(NT, 128, 32)
    m = (g[:, None].astype(np.int32) * g[None, :].astype(np.int32)) % SEQ
    k = np.arange(SEQ, dtype=np.float64)
    sc = 1.0 / np.sqrt(SEQ * 64.0)
    lc = (np.cos(2 * np.pi * k / SEQ) * sc).astype(np.float32).astype(bf)
    ls = (-np.sin(2 * np.pi * k / SEQ) * sc).astype(np.float32).astype(bf)
    tab = np.empty((128, NT, 2, SEQ), bf)
    tab[:, :, 0, :] = lc[m].reshape(NT, 128, SEQ).transpose(1, 0, 2)
    tab[:, :, 1, :] = ls[m].reshape(NT, 128, SEQ).transpose(1, 0, 2)
    del m
    j = np.arange(128)[:, None]
    i = np.arange(128)[None, :]
    prev = (j >= i).astype(np.float32)
    nxt = (j <= i).astype(np.float32)
    z = np.zeros((128, 128), np.float32)
    if s == 0:
        ms = [prev, nxt, z, prev, nxt, z]
    else:
        ms = [prev, nxt, prev, z, z, nxt]
    masks = np.stack(ms, axis=1).astype(bf)
    out = dict(ropec=rc, ropes=rs, tab=tab, masks=masks)
    _CONST_CACHE[key] = out
    return out


def _shared_consts():
    if "shared" in _CONST_CACHE:
        return _CONST_CACHE["shared"]
    bf = ml_dtypes.bfloat16
    n = np.arange(256, dtype=np.float64)
    a = 2 * np.pi * ((n[:, None] * n[None, :]) % 256) / 256.0
    sc = 1.0 / np.sqrt(256 * 64.0)
    tc = np.empty((128, 2, 2, 256), bf)
    tc[:, :, 0, :] = (np.cos(a) * sc).astype(np.float32).reshape(2, 128, 256).transpose(1, 0, 2).astype(bf)
    tc[:, :, 1, :] = (-np.sin(a) * sc).astype(np.float32).reshape(2, 128, 256).transpose(1, 0, 2).astype(bf)
    d = np.arange(64, dtype=np.float64)
    a64 = 2 * np.pi * ((d[:, None] * d[None, :]) % 64) / 64.0
    c64 = np.zeros((128, 128), np.float32)
    s64 = np.zeros((128, 128), np.float32)
    for gI in range(2):
        c64[gI * 64:(gI + 1) * 64, gI * 64:(gI + 1) * 64] = np.cos(a64)
        s64[gI * 64:(gI + 1) * 64, gI * 64:(gI + 1) * 64] = np.sin(a64)
    tp_ = np.arange(128)
    ltri = (tp_[:, None] < tp_[None, :]).astype(np.float32).astype(bf)
    out = dict(tabc=tc, c64bd=c64, s64bd=s64, ident_b=np.eye(128, dtype=np.float32).astype(bf),
               ident_f=np.eye(128, dtype=np.float32), ltri=ltri)
    _CONST_CACHE["shared"] = out
    return out


def prep(inp, cfg):
    f32 = np.float32
    A = lambda k_: np.ascontiguousarray(np.asarray(inp[k_], dtype=f32))
    x, cc, ctx, c_ctx = A("x"), A("c"), A("ctx"), A("c_ctx")
    SEQ = cfg.SEQ
    sh = dict(_shared_consts())
    NGh, RPDh, NFCh = cfg.NFC // cfg.FG, cfg.D_FF // 512, cfg.NFC
    pp = np.arange(128)[:, None]
    cW = np.zeros((128, 8 * NGh), np.float32)
    for ci in range(8):
        for g_ in range(NGh):
            cW[:, ci * NGh + g_] = (ci * 128 + pp[:, 0]) * RPDh + g_
    cD = np.zeros((128, NFCh), np.float32)
    for j in range(NFCh):
        cD[:, j] = j * 128 + pp[:, 0]
    sh["constW"], sh["constD"] = cW, cD
    sh["w_ada"] = A("w_ada")
    sh["badaT"] = np.ascontiguousarray(A("b_ada").reshape(2, 48, 128).transpose(0, 2, 1))
    gm, gf = A("g_mix_pre"), A("g_ffn_pre")
    gpreT = np.stack([gm.reshape(2, 8, 128), gf.reshape(2, 8, 128)], axis=1)
    sh["gpreT"] = np.ascontiguousarray(gpreT.transpose(0, 3, 1, 2))
    gpost = np.stack([A("g_mix_post"), A("g_ffn_post")], axis=1)
    sh["gpost"] = np.ascontiguousarray(np.broadcast_to(gpost[:, :, None, :], (2, 2, 128, 1024)))
    w_in = A("w_in").copy()
    q = w_in[:, :, 768:1280].reshape(2, 1024, 8, 64)
    order = [0, 4, 1, 5, 2, 6, 3, 7]
    w_in[:, :, 768:1280] = q[:, :, order, :].reshape(2, 1024, 512)
    sh["w_in"] = w_in
    sh["wsT"] = np.ascontiguousarray(A("w_s").transpose(0, 1, 3, 2))
    sh["bsT"] = np.ascontiguousarray(A("b_s").transpose(0, 2, 1))
    sh["gvb"] = np.ascontiguousarray(np.broadcast_to(A("g_v").reshape(2, 1, 256), (2, 128, 256)))
    sh["wf"] = A("w_f").reshape(2, 256, 64)
    sh["sinkb"] = np.ascontiguousarray(np.broadcast_to(A("sink").reshape(2, 1, 8), (2, 128, 8)))
    sh["w_out"] = A("w_out")
    sh["wg_d"], sh["wu_d"], sh["wd_d"] = A("w_gate_d")[0], A("w_up_d")[0], A("w_down_d")[0]
    sh["w_r"] = A("w_router")[0]
    sh["b_rb"] = np.ascontiguousarray(np.broadcast_to(A("b_router")[0].reshape(1, 8), (128, 8)))
    sh["wg_e"], sh["wu_e"], sh["wd_e"] = A("w_gate_e")[0], A("w_up_e")[0], A("w_down_e")[0]
    maps = []
    for core in range(cfg.NCORES):
        b, s = core // 2, core % 2
        m = dict(sh)
        m.update(_consts(cfg, s))
        m["xb"] = np.ascontiguousarray(np.roll(x[b], -s * (SEQ // 2), axis=0))
        m["ctxb"] = np.ascontiguousarray(ctx[b])
        cv = np.stack([cc[b].reshape(8, 128), c_ctx.reshape(8, 128)], axis=-1)
        m["cvecT"] = np.ascontiguousarray(cv.transpose(1, 0, 2))
        maps.append(m)
    return maps


_NC_CACHE = {}


def run(inp, cfg):
    key = (cfg.SEQ, cfg.D_FF, cfg.BATCH)
    if key not in _NC_CACHE:
        _NC_CACHE[key] = build(cfg)
    nc = _NC_CACHE[key]
    maps = prep(inp, cfg)
    res = run_bass_kernel_spmd(nc, maps, core_ids=list(range(cfg.NCORES)))
    SEQ = cfg.SEQ
    outp = np.empty((cfg.BATCH, SEQ, 1024), np.float32)
    for core in range(cfg.NCORES):
        b, s = core // 2, core % 2
        outp[b, s * (SEQ // 2):(s + 1) * (SEQ // 2)] = res.results[core]["out"]
    return outp


def kernel(**inputs):
    return run(inputs, Cfg())
```

```python
import numpy as np
import ml_dtypes
import concourse.bass as bass
import concourse.mybir as mybir
from concourse.bass_utils import run_bass_kernel_spmd

F32 = mybir.dt.float32
BF16 = mybir.dt.bfloat16
I32 = mybir.dt.int32
AF = mybir.ActivationFunctionType
ALU = mybir.AluOpType
AX = mybir.AxisListType

EPOCH = 30000
DMA_RING = 24


class Buf:
    __slots__ = ("name", "ap", "t", "last_w", "readers", "excl")

    def __init__(self, name, t):
        self.name = name
        self.t = t
        self.ap = t.ap() if hasattr(t, "ap") and callable(t.ap) else t
        self.last_w = None
        self.readers = {}
        self.excl = False


class Ctx:
    ENG = ("pe", "act", "dve", "pool", "sp")

    def __init__(self, nc):
        self.nc = nc
        self.prog = {e: [] for e in self.ENG}
        self.cnt = {e: 0 for e in self.ENG}
        self.sem = {}
        self.nsem = 0
        for e in ("pe", "act", "dve", "pool"):
            self.sem[e] = self._new_sem(e)
        self.ring = {"sp": DMA_RING, "pool": 40, "act": 2}
        self.dma_sems = {q: [self._new_sem("d" + q) for _ in range(self.ring[q])] for q in ("sp", "pool", "act")}
        self.dma_k = {q: 0 for q in ("sp", "pool", "act")}
        self.known = {e: {} for e in self.ENG}
        self.out_events = []
        self.n_ops = 0
        self.pending = {}

    def _new_sem(self, tag):
        self.nsem += 1
        return self.nc.alloc_semaphore(name="s_%s_%d" % (tag, self.nsem))

    def dram_in(self, name, shape, dt):
        return Buf(name, self.nc.dram_tensor(name, list(shape), dt, kind="ExternalInput"))

    def dram_out(self, name, shape, dt):
        return Buf(name, self.nc.dram_tensor(name, list(shape), dt, kind="ExternalOutput"))

    def dram(self, name, shape, dt):
        return Buf(name, self.nc.dram_tensor(name, list(shape), dt, kind="Internal"))

    def sb(self, name, shape, dt):
        return Buf(name, self.nc.alloc_sbuf_tensor(name, list(shape), dt))

    def ps(self, name, shape, dt=F32):
        b = Buf(name, self.nc.alloc_psum_tensor(name, list(shape), dt))
        b.excl = True
        return b

    def _deps(self, reads, writes):
        deps = {}

        def add(ev):
            if ev is None:
                return
            s, v = ev
            k = id(s)
            if k not in deps or deps[k][1] < v:
                deps[k] = (s, v)
        for b in reads:
            add(b.last_w)
        for b in writes:
            add(b.last_w)
            for ev in b.readers.values():
                add(ev)
        return list(deps.values())

    def _record(self, ev, reads, writes):
        for b in reads:
            if b in writes:
                continue
            b.readers[id(ev[0])] = ev
        for b in writes:
            b.last_w = ev
            b.readers = {}

    def _waits(self, eng, deps):
        kn = self.known[eng]
        out = []
        for s, v in deps:
            k = id(s)
            if kn.get(k, 0) >= v:
                continue
            kn[k] = v
            out.append((s, v))
        return out

    def _all_events(self):
        evs = []
        for q in self.dma_sems:
            k = self.dma_k[q]
            R_ = self.ring[q]
            for j in range(min(k, R_)):
                n_on_j = (k - 1 - j) // R_ + 1
                evs.append((self.dma_sems[q][j], 16 * n_on_j))
        for e in ("pe", "act", "dve", "pool"):
            if self.cnt[e] > 0:
                evs.append((self.sem[e], self.cnt[e]))
        return evs

    def barrier(self):
        evs = self._all_events()
        for e in self.ENG:
            self.pending[e] = list(evs)

    def op(self, eng, fn, reads=(), writes=(), nosync_same=False):
        ex = [b for b in reads if b.excl]
        if ex:
            writes = list(writes) + [b for b in ex if b not in writes]
        deps = self._deps(reads, writes)
        if nosync_same:
            deps = [d for d in deps if d[0] is not self.sem[eng]]
        deps += self.pending.pop(eng, [])
        waits = self._waits(eng, deps)
        if self.cnt[eng] >= EPOCH:
            self.sem[eng] = self._new_sem(eng)
            self.cnt[eng] = 0
        self.cnt[eng] += 1
        ev = (self.sem[eng], self.cnt[eng])
        self.prog[eng].append((waits, fn, ev[0], 1))
        self._record(ev, reads, writes)
        self.n_ops += 1
        return ev

    def dma(self, q, dst_buf, out_ap, in_ap, reads=(), writes=None, **kw):
        if writes is None:
            writes = [dst_buf]
        deps = self._deps(reads, writes)
        k = self.dma_k[q]
        self.dma_k[q] += 1
        R_ = self.ring[q]
        s = self.dma_sems[q][k % R_]
        v = 16 * (k // R_ + 1)
        if k >= R_:
            deps.append((s, v - 16))
        deps += self.pending.pop(q, [])
        waits = self._waits(q, deps)
        ev = (s, v)
        self.prog[q].append((waits, (lambda e, o=out_ap, i=in_ap, kw=kw: e.dma_start(out=o, in_=i, **kw)), s, 16))
        self._record(ev, reads, writes)
        self.n_ops += 1
        return ev

    def dma_custom(self, q, fn, reads=(), writes=()):
        deps = self._deps(reads, writes)
        k = self.dma_k[q]
        self.dma_k[q] += 1
        R_ = self.ring[q]
        s = self.dma_sems[q][k % R_]
        v = 16 * (k // R_ + 1)
        if k >= R_:
            deps.append((s, v - 16))
        deps += self.pending.pop(q, [])
        waits = self._waits(q, deps)
        ev = (s, v)
        self.prog[q].append((waits, fn, s, 16))
        self._record(ev, reads, writes)
        self.n_ops += 1
        return ev

    def mark_output(self, ev):
        self.out_events.append(ev)

    def finish(self, out_bufs=()):
        finals = list(self.out_events) + self._all_events()
        final_waits = self._waits("sp", finals)
        prog = self.prog
        nc = self.nc
        engmap = {"pe": "tensor", "act": "scalar", "dve": "vector", "pool": "gpsimd", "sp": "sync"}

        def replay(name):
            def body(e):
                for waits, fn, s, inc in prog[name]:
                    for ws, wv in waits:
                        e.wait_ge(ws, wv)
                    fn(e).then_inc(s, inc)
                if name == "sp":
                    for ws, wv in final_waits:
                        e.wait_ge(ws, wv)
            return body
        with nc.Block() as block:
            for name in self.ENG:
                getattr(block, engmap[name])(replay(name))


class Cfg:
    def __init__(self, SEQ=8192, D_FF=3584, BATCH=4):
        self.SEQ, self.D_FF, self.BATCH = SEQ, D_FF, BATCH
        self.D = 1024
        self.CTX = 256
        self.NE = 8
        self.NT = SEQ // 128
        self.NH = self.NT // 2
        self.NFC = D_FF // 128
        self.FG = 4
        assert self.NFC % self.FG == 0
        self.NCORES = 2 * BATCH


REC_W = 832


class _Stop(Exception):
    pass


STOP_AT = None
SPARSE_MOE = True
PRECONV = True


def build(cfg):
    def chk(tag):
        if STOP_AT == tag:
            raise _Stop()
    nc = bass.Bass("TRN2", target_bir_lowering=False)
    c = Ctx(nc)
    D, NT, NH, DFF, NFC, FG, NE = cfg.D, cfg.NT, cfg.NH, cfg.D_FF, cfg.NFC, cfg.FG, cfg.NE
    SEQ = cfg.SEQ
    NG = NFC // FG

    di = c.dram_in
    xb = di("xb", [SEQ, D], F32)
    ctxb = di("ctxb", [256, D], F32)
    cvecT = di("cvecT", [128, 8, 2], F32)
    w_ada = di("w_ada", [2, D, 6 * D], F32)
    badaT = di("badaT", [2, 128, 48], F32)
    gpreT = di("gpreT", [2, 128, 2, 8], F32)
    gpost = di("gpost", [2, 2, 128, D], F32)
    w_in = di("w_in", [2, D, 1536], F32)
    wsT = di("wsT", [2, 4, 128, 128], F32)
    bsT = di("bsT", [2, 128, 4], F32)
    gvb = di("gvb", [2, 128, 256], F32)
    wf = di("wf", [2, 256, 64], F32)
    sinkb = di("sinkb", [2, 128, 8], F32)
    w_out = di("w_out", [2, D, D], F32)
    wg_d = di("wg_d", [D, DFF], F32)
    wu_d = di("wu_d", [D, DFF], F32)
    wd_d = di("wd_d", [DFF, D], F32)
    w_r = di("w_r", [D, NE], F32)
    b_rb = di("b_rb", [128, NE], F32)
    wg_e = di("wg_e", [NE, D, DFF], F32)
    wu_e = di("wu_e", [NE, D, DFF], F32)
    wd_e = di("wd_e", [NE, DFF, D], F32)
    ident_bd = di("ident_b", [128, 128], BF16)
    ident_fd = di("ident_f", [128, 128], F32)
    ropec = di("ropec", [NT + 1, 128, 32], F32)
    ropes = di("ropes", [NT + 1, 128, 32], F32)
    tab = di("tab", [128, NT, 2, SEQ], BF16)
    tabc = di("tabc", [128, 2, 2, 256], BF16)
    c64d = di("c64bd", [128, 128], F32)
    s64d = di("s64bd", [128, 128], F32)
    masksd = di("masks", [128, 6, 128], BF16)
    out = c.dram_out("out", [SEQ // 2, D], F32)
    NHt = NH
    T_OWN = NH * 128
    NS = 2 * T_OWN // 512 + NE
    RPD = DFF // 512
    ltrid = di("ltri", [128, 128], BF16)
    constWd = di("constW", [128, 8 * NG], F32)
    constDd = di("constD", [128, NFC], F32)
    WGB = c.dram("WGB", [NG, 128, 8, FG * 128], BF16)
    WUB = c.dram("WUB", [NG, 128, 8, FG * 128], BF16)
    WDB = c.dram("WDB", [NG, 128, FG, D], BF16)
    Y2 = c.dram("Y2", [NHt, 128, D], BF16)
    Y2S = c.dram("Y2S", [NS * 512, D], BF16)
    RS = c.dram("RS", [NS * 512, D], F32)

    REC = c.dram("REC", [NT, 128, REC_W], BF16)
    OAT = c.dram("OAT", [NT, 128, 256], BF16)
    HM = c.dram("HM", [NT, 128, D], F32)
    H1 = c.dram("H1", [NT, 128, D], F32)
    HCM = c.dram("HCM", [2, 128, D], F32)
    HC1 = c.dram("HC1", [2, 128, D], F32)

    st = {"off": 16384}

    def sb(name, shape, dt):
        esz = 4 if dt in (F32, I32) else 2
        n = 1
        for s_ in shape[1:]:
            n *= s_
        nbytes = (n * esz + 31) // 32 * 32
        t = nc.alloc_sbuf_tensor_at(name, list(shape), dt, offset=st["off"])
        st["off"] += nbytes
        assert st["off"] <= 16384 + 212000, ("SBUF overflow", name, st["off"])
        return Buf(name, t)

    banks = [c.ps("bank%d" % i, [128, 512], F32) for i in range(8)]

    def bfv(bank):
        return bank.ap.bitcast(BF16)

    ident_b = sb("ident_b", [128, 128], BF16)
    ident_f = sb("ident_f", [128, 128], F32)
    ones_f = sb("ones_f", [128, 128], F32)
    masks = sb("masks", [128, 6, 128], BF16)
    c64 = sb("c64", [128, 128], F32)
    s64 = sb("s64", [128, 128], F32)
    cT = sb("cT", [128, 8, 2], F32)
    c.dma("sp", ident_b, ident_b.ap[:], ident_bd.ap[:])
    c.dma("sp", ident_f, ident_f.ap[:], ident_fd.ap[:])
    c.dma("sp", masks, masks.ap[:], masksd.ap[:])
    c.dma("sp", c64, c64.ap[:], c64d.ap[:])
    c.dma("sp", s64, s64.ap[:], s64d.ap[:])
    c.dma("sp", cT, cT.ap[:], cvecT.ap[:])
    c.op("dve", lambda e: e.memset(ones_f.ap[:], 1.0), writes=[ones_f])
    scT = sb("scT", [128, 8, 2], F32)
    c.op("act", lambda e: e.activation(scT.ap[:], cT.ap[:], AF.Silu), reads=[cT], writes=[scT])

    adaT = sb("adaT", [128, 48, 2], F32)
    bada = sb("bada", [128, 48], F32)
    gpre = sb("gpre", [128, 2, 8], F32)
    MUL = sb("MUL", [128, 2, 2, 8], F32)
    G = [[sb("G%d%d" % (a, b), [128, D], F32) for b in range(2)] for a in range(2)]
    wsTb = sb("wsTb", [128, 4, 128], BF16)
    bsTs = sb("bsTs", [128, 4], F32)
    gvs = sb("gvs", [128, 256], F32)
    CW = sb("CW", [128, 2, 2, 128], BF16)
    esink = sb("esink", [128, 512], F32)
    recC = [sb("recC%d" % i, [128, REC_W], BF16) for i in range(2)]
    MULbc = sb("MULbc", [128, D], F32)
    ADDbc = sb("ADDbc", [128, D], F32)
    tabc_sb = sb("tabc_sb", [128, 2, 2, 256], BF16)
    c.dma("sp", tabc_sb, tabc_sb.ap[:], tabc.ap[:])
    persistent_end = st["off"]

    def norm_bufs(tag):
        return dict(
            ht=sb(tag + "ht", [128, D], F32),
            junk=sb(tag + "junk", [128, D], BF16),
            ss=sb(tag + "ss", [128, 1], F32),
            rs=sb(tag + "rs", [128, 1], F32),
            xn=sb(tag + "xn", [128, D], BF16),
        )

    def emit_norm_T(nb, who, which, ymT_ap_fn, ymT_buf, tbank):
        ht, junk, ss, rs, xn = nb["ht"], nb["junk"], nb["ss"], nb["rs"], nb["xn"]
        c.op("pool", lambda e: e.memset(ss.ap[:], 0.0), writes=[ss])
        c.op("act", lambda e: e.activation(junk.ap[:], ht.ap[:], AF.Square, accum_out=ss.ap[:, 0:1]),
             reads=[ht], writes=[junk, ss])
        c.op("act", lambda e: e.activation(rs.ap[:], ss.ap[:], AF.Sqrt, bias=1e-6, scale=1.0 / D),
             reads=[ss], writes=[rs])
        c.op("dve", lambda e: e.reciprocal(rs.ap[:], rs.ap[:]), reads=[rs], writes=[rs])
        c.op("dve", lambda e: e.tensor_scalar(xn.ap[:], ht.ap[:], rs.ap[:, 0:1], None, ALU.mult),
             reads=[ht, rs], writes=[xn])
        tv = bfv(tbank)
        for ci in range(8):
            c.op("pe", lambda e, ci=ci: e.transpose(tv[:, ci * 128:(ci + 1) * 128], xn.ap[:, ci * 128:(ci + 1) * 128], ident_b.ap[:]),
                 reads=[xn, ident_b], writes=[tbank], nosync_same=True)
        for ci in range(8):
            eng = "dve" if ci % 2 == 0 else "act"
            mul_ap = MUL.ap[:, which, who, ci:ci + 1]
            add_ap = adaT.ap[:, (0 if which == 0 else 3) * 8 + ci, who:who + 1]
            if eng == "dve":
                c.op("dve", lambda e, ci=ci, m=mul_ap, a=add_ap: e.tensor_scalar(
                    ymT_ap_fn(ci), tv[:, ci * 128:(ci + 1) * 128], m, a, ALU.mult, ALU.add),
                    reads=[tbank, MUL, adaT], writes=[ymT_buf])
            else:
                c.op("act", lambda e, ci=ci, m=mul_ap, a=add_ap: e.activation(
                    ymT_ap_fn(ci), tv[:, ci * 128:(ci + 1) * 128], AF.Identity, bias=a, scale=m),
                    reads=[tbank, MUL, adaT], writes=[ymT_buf])

    def layer(L):
        last = (L == 1)
        st["off"] = persistent_end
        c.barrier()
        c.dma("sp", bada, bada.ap[:], badaT.ap[L])
        c.dma("sp", gpre, gpre.ap[:], gpreT.ap[L])
        wa = [sb("wa%d" % i, [128, 8, 512], F32) for i in range(2)]
        wav = w_ada.ap[L].rearrange("(c p) n -> p c n", p=128)
        pb = banks[0]
        for grp in range(12):
            wb = wa[grp % 2]
            c.dma("sp", wb, wb.ap[:], wav[:, :, grp * 512:(grp + 1) * 512])
            for jj in range(4):
                j = grp * 4 + jj
                for ci in range(8):
                    c.op("pe", lambda e, wb=wb, jj=jj, j=j, ci=ci: e.matmul(
                        pb.ap[:, 2 * j:2 * j + 2], wb.ap[:, ci, jj * 128:(jj + 1) * 128], scT.ap[:, ci, :],
                        start=(ci == 0), stop=(ci == 7)), reads=[wb, scT], writes=[pb], nosync_same=True)
        for v in range(2):
            c.op("dve", lambda e, v=v: e.tensor_tensor(
                adaT.ap[:, :, v], pb.ap[:, 0:96].rearrange("p (j v) -> p j v", v=2)[:, :, v], bada.ap[:], ALU.add),
                reads=[pb, bada], writes=[adaT])
        for which in range(2):
            for who in range(2):
                scv = adaT.ap[:, (1 if which == 0 else 4) * 8:(1 if which == 0 else 4) * 8 + 8, who]
                c.op("dve", lambda e, which=which, who=who, scv=scv: e.scalar_tensor_tensor(
                    MUL.ap[:, which, who, :], scv, 1.0, gpre.ap[:, which, :], ALU.add, ALU.mult),
                    reads=[adaT, gpre], writes=[MUL])
        gp = sb("gp", [128, D], F32)
        bc = sb("bcst", [128, 128], F32)
        for which in range(2):
            c.dma("sp", gp, gp.ap[:], gpost.ap[L, which])
            for who in range(2):
                for ci in range(8):
                    col = adaT.ap[:, (2 if which == 0 else 5) * 8 + ci, who:who + 1]
                    c.op("dve", lambda e, col=col: e.tensor_scalar(bc.ap[:], ones_f.ap[:], col, None, ALU.mult),
                         reads=[ones_f, adaT], writes=[bc])
                    bk = banks[1 + ci // 4]
                    c.op("pe", lambda e, bk=bk, ci=ci: e.matmul(
                        bk.ap[:, (ci % 4) * 128:(ci % 4 + 1) * 128], bc.ap[:], ident_f.ap[:], start=True, stop=True),
                        reads=[bc, ident_f], writes=[bk], nosync_same=True)
                for hh in range(2):
                    c.op("dve", lambda e, hh=hh, which=which, who=who: e.tensor_tensor(
                        G[which][who].ap[:, hh * 512:(hh + 1) * 512], banks[1 + hh].ap[:], gp.ap[:, hh * 512:(hh + 1) * 512], ALU.mult),
                        reads=[banks[1 + hh], gp], writes=[G[which][who]])
        if L == 1 and SPARSE_MOE:
            for dst_t, colfn in ((MULbc, lambda ci: MUL.ap[:, 1, 0, ci:ci + 1]), (ADDbc, lambda ci: adaT.ap[:, 3 * 8 + ci, 0:1])):
                for ci in range(8):
                    col = colfn(ci)
                    c.op("dve", lambda e, col=col: e.tensor_scalar(bc.ap[:], ones_f.ap[:], col, None, ALU.mult),
                         reads=[ones_f, adaT, MUL], writes=[bc])
                    bk = banks[1 + ci // 4]
                    c.op("pe", lambda e, bk=bk, ci=ci: e.matmul(
                        bk.ap[:, (ci % 4) * 128:(ci % 4 + 1) * 128], bc.ap[:], ident_f.ap[:], start=True, stop=True),
                        reads=[bc, ident_f], writes=[bk], nosync_same=True)
                for hh in range(2):
                    c.op("act", lambda e, hh=hh, dst_t=dst_t: e.copy(dst_t.ap[:, hh * 512:(hh + 1) * 512], banks[1 + hh].ap[:]),
                         reads=[banks[1 + hh]], writes=[dst_t])
        for h in range(4):
            c.dma("pool", wsTb, wsTb.ap[:, h, :], wsT.ap[L, h])
        c.dma("sp", bsTs, bsTs.ap[:], bsT.ap[L])
        c.dma("sp", gvs, gvs.ap[:], gvb.ap[L])
        sk = sb("sk", [128, 8], F32)
        c.dma("sp", sk, sk.ap[:], sinkb.ap[L])
        c.op("act", lambda e: e.activation(sk.ap[:], sk.ap[:], AF.Exp), reads=[sk], writes=[sk])
        c.op("dve", lambda e: e.tensor_copy(
            esink.ap[0:64, :].rearrange("p (c q) -> p c q", q=128), sk.ap[0:64, 4:8].unsqueeze(2).broadcast_to([64, 4, 128])),
            reads=[sk], writes=[esink])
        c.op("dve", lambda e: e.tensor_copy(
            esink.ap[64:128, :].rearrange("p (c q) -> p c q", q=128), sk.ap[64:128, 0:4].unsqueeze(2).broadcast_to([64, 4, 128])),
            reads=[sk], writes=[esink])
        wfs = sb("wfs", [128, 2, 64], F32)
        c.dma("sp", wfs, wfs.ap[:], wf.ap[L].rearrange("(j p) e -> p j e", p=128))
        c.op("pool", lambda e: e.memset(CW.ap[:], 0.0), writes=[CW])
        for part, cs in enumerate((c64, s64)):
            for j in range(2):
                bk = banks[3]
                c.op("pe", lambda e, cs=cs, j=j, bk=bk: e.matmul(bk.ap[:, 0:64], cs.ap[:], wfs.ap[:, j, :], start=True, stop=True),
                     reads=[cs, wfs], writes=[bk], nosync_same=True)
                c.op("dve", lambda e, part=part, j=j, bk=bk: e.tensor_copy(CW.ap[0:64, part, j, 0:64], bk.ap[0:64, 0:64]),
                     reads=[bk], writes=[CW])
                c.op("dve", lambda e, part=part, j=j, bk=bk: e.tensor_copy(CW.ap[64:128, part, j, 64:128], bk.ap[64:128, 0:64]),
                     reads=[bk], writes=[CW])
        c.barrier()
        chk("setup%d" % L)
        st["off"] = persistent_end
        mixer_top = st["off"]

        P_all = sb("P_all", [128, NT, 512], BF16)
        P_ctx = sb("P_ctx", [128, 2, 512], BF16)
        oatC = sb("oatC", [128, 2, 256], BF16)
        p12_top = st["off"]
        w_in_sb = sb("w_in_sb", [128, 8, 1536], BF16)
        c.dma("pool", w_in_sb, w_in_sb.ap[:], w_in.ap[L].rearrange("(c p) n -> p c n", p=128))
        nbs = [norm_bufs("p1%d" % i) for i in range(2)]
        NSET = 3
        wk = []
        for i in range(NSET):
            d_ = dict(
                ymT=sb("ymT%d" % i, [128, 8, 128], BF16),
                ga=sb("ga%d" % i, [128, 512], F32),
                sq=sb("sq%d" % i, [128, 256], F32),
                ssv=sb("ssv%d" % i, [128, 4], F32),
                vn=sb("vn%d" % i, [128, 256], BF16),
                oA=sb("oA%d" % i, [128, 256], BF16),
                oAT=sb("oAT%d" % i, [128, 256], BF16),
                fbT=sb("fbT%d" % i, [128, 2, 128], BF16),
                zq=sb("zq%d" % i, [128, 640], F32),
                t1=sb("t1%d" % i, [128, 320], F32),
                t2=sb("t2%d" % i, [128, 320], F32),
                qkr=sb("qkr%d" % i, [128, 640], BF16),
                rec=sb("rec%d" % i, [128, REC_W], BF16),
                rc=sb("rc%d" % i, [128, 32], F32),
                rsn=sb("rsn%d" % i, [128, 32], F32),
            )
            c.op("pool", lambda e, d_=d_: e.memset(d_["rec"].ap[:, 704:768], 1.0), writes=[d_["rec"]])
            wk.append(d_)
        for i in range(2):
            c.op("pool", lambda e, i=i: e.memset(recC[i].ap[:, 704:768], 1.0), writes=[recC[i]])

        cnt = {"i": 0}

        def pass1_tile(src_ap, who, rope_idx, fA, fB, fQ, fKV, P_dst_buf, P_dst_ap, rec_sink, oat_sink):
            par = cnt["i"] % 2
            nb, w = nbs[par], wk[cnt["i"] % NSET]
            cnt["i"] += 1
            ymT = w["ymT"]
            c.dma("sp", nb["ht"], nb["ht"].ap[:], src_ap)
            emit_norm_T(nb, who, 0, lambda ci: ymT.ap[:, ci, :], ymT, banks[0])
            chk("p1a")
            def back():
                if fA:
                    zA = banks[1]
                    for ci in range(8):
                        c.op("pe", lambda e, ci=ci: e.matmul(zA.ap[:], ymT.ap[:, ci, :], w_in_sb.ap[:, ci, 0:512], start=(ci == 0), stop=(ci == 7)),
                             reads=[ymT, w_in_sb], writes=[zA], nosync_same=True)
                    ga = w["ga"]
                    c.op("act", lambda e: e.activation(ga.ap[:], zA.ap[:], AF.Gelu), reads=[zA], writes=[ga])
                    sq, ssv, vn, oA, oAT = w["sq"], w["ssv"], w["vn"], w["oA"], w["oAT"]
                    c.op("pool", lambda e: e.tensor_tensor(sq.ap[:], ga.ap[:, 256:512], ga.ap[:, 256:512], ALU.mult), reads=[ga], writes=[sq])
                    c.op("dve", lambda e: e.tensor_reduce(ssv.ap[:], sq.ap[:].rearrange("p (h d) -> p h d", d=64), AX.X, ALU.add), reads=[sq], writes=[ssv])
                    c.op("act", lambda e: e.activation(ssv.ap[:], ssv.ap[:], AF.Sqrt, bias=1e-6, scale=1.0 / 64), reads=[ssv], writes=[ssv])
                    c.op("dve", lambda e: e.reciprocal(ssv.ap[:], ssv.ap[:]), reads=[ssv], writes=[ssv])
                    for h in range(4):
                        c.op("dve", lambda e, h=h: e.scalar_tensor_tensor(
                            vn.ap[:, h * 64:(h + 1) * 64], ga.ap[:, 256 + h * 64:256 + (h + 1) * 64], ssv.ap[:, h:h + 1],
                            gvs.ap[:, h * 64:(h + 1) * 64], ALU.mult, ALU.mult), reads=[ga, ssv, gvs], writes=[vn])
                    yield
                    svb = banks[5]
                    for h in range(4):
                        c.op("pe", lambda e, h=h: e.matmul(svb.ap[:, h * 64:(h + 1) * 64], wsTb.ap[:, h, :], vn.ap[:, h * 64:(h + 1) * 64], start=True, stop=True),
                             reads=[wsTb, vn], writes=[svb], nosync_same=True)
                    for h in range(4):
                        c.op("dve", lambda e, h=h: e.scalar_tensor_tensor(
                            oA.ap[:, h * 64:(h + 1) * 64], svb.ap[:, h * 64:(h + 1) * 64], bsTs.ap[:, h:h + 1],
                            ga.ap[:, h * 64:(h + 1) * 64], ALU.add, ALU.mult), reads=[svb, bsTs, ga], writes=[oA])
                    tb = banks[7]
                    tvv = bfv(tb)
                    for j in range(2):
                        c.op("pe", lambda e, j=j: e.transpose(tvv[:, 640 + j * 128:640 + (j + 1) * 128], oA.ap[:, j * 128:(j + 1) * 128], ident_b.ap[:]),
                             reads=[oA, ident_b], writes=[tb], nosync_same=True)
                    if oat_sink[0] == "dram":
                        c.op("act", lambda e: e.copy(oAT.ap[:], tvv[:, 640:896]), reads=[tb], writes=[oAT])
                        c.dma("pool", OAT, oat_sink[1], oAT.ap[:], reads=[oAT], writes=[])
                    else:
                        c.op("act", lambda e: e.copy(oat_sink[1], tvv[:, 640:896]), reads=[tb], writes=[oat_sink[2]])
                chk("p1b")
                yield
                if fB:
                    zb = banks[2]
                    fbT = w["fbT"]
                    for j in range(2):
                        for ci in range(8):
                            c.op("pe", lambda e, j=j, ci=ci: e.matmul(
                                zb.ap[:, j * 128:(j + 1) * 128], w_in_sb.ap[:, ci, 512 + j * 128:512 + (j + 1) * 128], ymT.ap[:, ci, :],
                                start=(ci == 0), stop=(ci == 7)), reads=[ymT, w_in_sb], writes=[zb], nosync_same=True)
                    c.op("act", lambda e: e.copy(fbT.ap[:].rearrange("p j t -> p (j t)"), zb.ap[:, 0:256]), reads=[zb], writes=[fbT])
                    pbk = banks[6]
                    for part in range(2):
                        for j in range(2):
                            c.op("pe", lambda e, part=part, j=j: e.matmul(
                                pbk.ap[:, part * 256 + j * 128:part * 256 + (j + 1) * 128], fbT.ap[:, j, :], CW.ap[:, part, j, :], start=True, stop=True),
                                reads=[fbT, CW], writes=[pbk], nosync_same=True)
                    c.op("dve", lambda e: e.tensor_copy(P_dst_ap, pbk.ap[:]), reads=[pbk], writes=[P_dst_buf])
                chk("p1c")
                if fQ or fKV:
                    rec = w["rec"] if rec_sink[0] == "dram" else rec_sink[1]
                    zq, t1, t2, qkr = w["zq"], w["t1"], w["t2"], w["qkr"]
                    rc, rsn = w["rc"], w["rsn"]
                    c.dma("sp", rc, rc.ap[:], ropec.ap[rope_idx])
                    c.dma("sp", rsn, rsn.ap[:], ropes.ap[rope_idx])
                    chk("p1d0")
                    b3, b4 = banks[3], banks[4]
                    lo = 0 if fQ else 512
                    if fQ:
                        for ci in range(8):
                            c.op("pe", lambda e, ci=ci: e.matmul(b3.ap[:], ymT.ap[:, ci, :], w_in_sb.ap[:, ci, 768:1280], start=(ci == 0), stop=(ci == 7)),
                                 reads=[ymT, w_in_sb], writes=[b3], nosync_same=True)
                        c.op("act", lambda e: e.copy(zq.ap[:, 0:512], b3.ap[:]), reads=[b3], writes=[zq])
                    chk("p1d1")
                    for ci in range(8):
                        c.op("pe", lambda e, ci=ci: e.matmul(b4.ap[:, 0:256], ymT.ap[:, ci, :], w_in_sb.ap[:, ci, 1280:1536], start=(ci == 0), stop=(ci == 7)),
                             reads=[ymT, w_in_sb], writes=[b4], nosync_same=True)
                    c.op("act", lambda e: e.copy(zq.ap[:, 512:640], b4.ap[:, 0:128]), reads=[b4], writes=[zq])
                    chk("p1d2")
                    c.op("dve", lambda e: e.tensor_copy(rec.ap[:, 640:704], b4.ap[:, 128:192]), reads=[b4], writes=[rec])
                    c.op("dve", lambda e: e.tensor_copy(rec.ap[:, 768:832], b4.ap[:, 192:256]), reads=[b4], writes=[rec])
                    chk("p1d")
                    yield
                    nh_ = (640 - lo) // 64

                    def v5(ap_, half):
                        return ap_.rearrange("p (h a f r) -> p h a f r", a=2, f=2, r=16)[:, :, :, half, :]

                    def tv(ap_):
                        return ap_.rearrange("p (h a r) -> p h a r", a=2, r=16)

                    def tb_(ap_):
                        return ap_.rearrange("p (a r) -> p a r", r=16).unsqueeze(1).broadcast_to([128, nh_, 2, 16])
                    x1 = v5(zq.ap[:, lo:640], 0)
                    x2 = v5(zq.ap[:, lo:640], 1)
                    o1 = v5(qkr.ap[:, lo:640], 0)
                    o2 = v5(qkr.ap[:, lo:640], 1)
                    T1 = tv(t1.ap[:, 0:nh_ * 32])
                    T2 = tv(t2.ap[:, 0:nh_ * 32])
                    cs_, sn_ = tb_(rc.ap[:]), tb_(rsn.ap[:])
                    c.op("dve", lambda e: e.tensor_tensor(T1, x1, cs_, ALU.mult), reads=[zq, rc], writes=[t1])
                    c.op("dve", lambda e: e.tensor_tensor(T2, x2, sn_, ALU.mult), reads=[zq, rsn], writes=[t2])
                    c.op("dve", lambda e: e.tensor_tensor(o1, T1, T2, ALU.subtract), reads=[t1, t2], writes=[qkr])
                    c.op("dve", lambda e: e.tensor_tensor(T1, x2, cs_, ALU.mult), reads=[zq, rc, qkr], writes=[t1])
                    c.op("dve", lambda e: e.tensor_tensor(T2, x1, sn_, ALU.mult), reads=[zq, rsn, qkr], writes=[t2])
                    c.op("dve", lambda e: e.tensor_tensor(o2, T1, T2, ALU.add), reads=[t1, t2], writes=[qkr])
                    chk("p1e")
                    tb = banks[7]
                    tvv = bfv(tb)
                    for j in range(lo // 128, 5):
                        c.op("pe", lambda e, j=j: e.transpose(tvv[:, j * 128:(j + 1) * 128], qkr.ap[:, j * 128:(j + 1) * 128], ident_b.ap[:]),
                             reads=[qkr, ident_b], writes=[tb], nosync_same=True)
                    c.op("act", lambda e: e.copy(rec.ap[:, lo:640], tvv[:, lo:640]), reads=[tb], writes=[rec])
                    if rec_sink[0] == "dram":
                        c.dma("pool", REC, rec_sink[1], rec.ap[:], reads=[rec], writes=[])
                    chk("p1f")
            return back()

        active = []

        def exhaust(g):
            for _ in g:
                pass

        def p1(*a):
            while len(active) >= NSET - 1:
                exhaust(active.pop(0))
            active.append(pass1_tile(*a))
            for g in list(active):
                try:
                    next(g)
                except StopIteration:
                    active.remove(g)
        for i in range(2):
            src = ctxb.ap[i * 128:(i + 1) * 128, :] if L == 0 else HC1.ap[i]
            if L == 0:
                p1(src, 1, NT, True, True, True, True, P_ctx, P_ctx.ap[:, i, :], ("sb", recC[i]), ("sb", oatC.ap[:, i, :], oatC))
            else:
                p1(src, 1, NT, False, False, False, True, None, None, ("sb", recC[i]), None)
        conv_jobs = []
        if L == 0 and PRECONV:
            cvb = [sb("cvb%d" % i, [128, 8, FG * 128], BF16) for i in range(1)]
            cvs = {"k": 0}

            def mk_job(kind, gi):
                def job():
                    b_ = cvb[0]
                    cvs["k"] += 1
                    cols = slice(gi * FG * 128, (gi + 1) * FG * 128)
                    if kind == 0:
                        c.dma("pool", b_, b_.ap[:], wg_d.ap.rearrange("(c p) n -> p c n", p=128)[:, :, cols])
                        c.dma("pool", WGB, WGB.ap[gi], b_.ap[:], reads=[b_], writes=[])
                    elif kind == 1:
                        c.dma("pool", b_, b_.ap[:], wu_d.ap.rearrange("(c p) n -> p c n", p=128)[:, :, cols])
                        c.dma("pool", WUB, WUB.ap[gi], b_.ap[:], reads=[b_], writes=[])
                    else:
                        bv = b_.ap[:].rearrange("p c n -> p (c n)").rearrange("p (f n) -> p f n", n=D)
                        c.dma("pool", b_, bv, wd_d.ap[gi * FG * 128:(gi + 1) * FG * 128, :].rearrange("(c p) n -> p c n", p=128))
                        c.dma("pool", WDB, WDB.ap[gi], bv, reads=[b_], writes=[])
                return job
            for gi in range(NG):
                for kind in range(3):
                    conv_jobs.append(mk_job(kind, gi))
        for t in range(NT):
            if conv_jobs and t % 2 == 0:
                conv_jobs.pop(0)()
            src = xb.ap[t * 128:(t + 1) * 128, :] if L == 0 else H1.ap[t]
            if L == 0 or t < NH:
                fl = (True, True, True, True)
            elif t == NH or t == NT - 1:
                fl = (False, True, False, True)
            else:
                fl = (False, True, False, False)
            p1(src, 0, t, fl[0], fl[1], fl[2], fl[3], P_all, P_all.ap[:, t, :], ("dram", REC.ap[t]), ("dram", OAT.ap[t]))
        while active:
            exhaust(active.pop(0))
        while conv_jobs:
            conv_jobs.pop(0)()
        c.barrier()
        chk("pass1_%d" % L)
        st["off"] = p12_top

        woAB = sb("woAB", [128, 4, D], BF16)
        woC = sb("woC", [128, 4, D], BF16)
        c.dma("pool", woAB, woAB.ap[:], w_out.ap[L, 0:512, :].rearrange("(c p) n -> p c n", p=128))
        for g in range(2):
            c.dma("pool", woC, woC.ap[64 * g:64 * g + 64, :, :],
                  w_out.ap[L, 512 + 256 * g:512 + 256 * g + 256, :].rearrange("(c d) n -> d c n", d=64))
        TG = 4
        tbufs = [sb("tbuf%d" % i, [128, TG, 2, 512], BF16) for i in range(4)]
        oBT = [sb("oBT%d" % i, [128, 2, 512], BF16) for i in range(2)]
        ring = [sb("ring%d" % i, [128, REC_W], BF16) for i in range(5)]
        oats = [sb("oats%d" % i, [128, 256], BF16) for i in range(2)]
        pTs = [sb("pT%d" % i, [128, 512], BF16) for i in range(3)]
        dn = [sb("dn%d" % i, [128, 512], F32) for i in range(2)]
        rcp = [sb("rcp%d" % i, [128, 512], F32) for i in range(2)]
        oCT = [sb("oCT%d" % i, [128, 512], BF16) for i in range(2)]
        hts = [sb("hts%d" % i, [128, D], F32) for i in range(2)]
        junk2 = [sb("junk2%d" % i, [128, D], BF16) for i in range(2)]
        ss2 = [sb("ss2%d" % i, [128, 2], F32) for i in range(2)]
        tmpo = [sb("tmpo%d" % i, [128, D], F32) for i in range(2)]
        ring_of = {}
        rst = {"k": 0, "pt": 0, "t": 0}

        def get_rec(t):
            t = t % NT
            if t in ring_of:
                return ring_of[t]
            slot = ring[rst["k"] % 5]
            rst["k"] += 1
            for k_, v_ in list(ring_of.items()):
                if v_ is slot:
                    del ring_of[k_]
            c.dma("sp", slot, slot.ap[:], REC.ap[t], reads=[REC])
            ring_of[t] = slot
            return slot

        def attention(qrec, kblocks, par):
            oc = oCT[par]
            nkb = len(kblocks)
            steps = [(g, bi, kr, mi) for g in range(2) for bi, (kr, mi) in enumerate(kblocks)]
            slots = []

            def emit_S(i):
                g, bi, kr, mi = steps[i]
                sbk = banks[2 + (rst["pt"] % 2)]
                pT = pTs[rst["pt"] % 3]
                rst["pt"] += 1
                slots.append(pT)
                c.op("pe", lambda e, kr=kr, sbk=sbk, g=g: e.matmul(
                    sbk.ap[:], kr.ap[64 * g:64 * g + 64, 512:640], qrec.ap[64 * g:64 * g + 64, 0:512], start=True, stop=True),
                    reads=[kr, qrec], writes=[sbk], nosync_same=True)
                c.op("act", lambda e, sbk=sbk, pT=pT: e.activation(pT.ap[:], sbk.ap[:], AF.Exp, scale=0.125), reads=[sbk], writes=[pT])
                if mi is not None:
                    c.op("dve", lambda e, pT=pT, mi=mi: e.tensor_tensor(
                        pT.ap[:].rearrange("p (c q) -> p c q", q=128), pT.ap[:].rearrange("p (c q) -> p c q", q=128),
                        masks.ap[:, mi, :].unsqueeze(1).broadcast_to([128, 4, 128]), ALU.mult), reads=[pT, masks], writes=[pT])

            def emit_PV(i):
                g, bi, kr, mi = steps[i]
                pT = slots[i]
                acc = banks[4 + g]
                c.op("pe", lambda e, kr=kr, pT=pT, acc=acc, g=g, bi=bi: e.matmul(
                    acc.ap[:], kr.ap[:, 640 + 64 * g:640 + 64 * g + 128], pT.ap[:], start=(bi == 0), stop=(bi == nkb - 1)),
                    reads=[kr, pT], writes=[acc], nosync_same=True)
                if bi == nkb - 1:
                    pn = slice(0, 64) if g == 0 else slice(64, 128)
                    pd = slice(64, 128) if g == 0 else slice(0, 64)
                    d_, r_ = dn[g], rcp[g]
                    c.op("dve", lambda e, acc=acc, pd=pd, d_=d_: e.tensor_tensor(d_.ap[pd, :], acc.ap[pd, :], esink.ap[pd, :], ALU.add),
                         reads=[acc, esink], writes=[d_])
                    c.op("dve", lambda e, pd=pd, pn=pn, d_=d_, r_=r_: e.reciprocal(r_.ap[pn, :], d_.ap[pd, :]), reads=[d_], writes=[r_])
                    c.op("dve", lambda e, acc=acc, pn=pn, r_=r_, oc=oc: e.tensor_tensor(oc.ap[pn, :], acc.ap[pn, :], r_.ap[pn, :], ALU.mult),
                         reads=[acc, r_], writes=[oc])
            n = len(steps)
            emit_S(0)
            for i in range(1, n):
                emit_S(i)
                emit_PV(i - 1)
            emit_PV(n - 1)
            return oc

        def out_proj(oat_buf, oat_ap, obt_buf, obt_ap_fn, oc, h_src_ap, h_src_reads, who, dst_buf, dst_ap):
            par = rst["t"] % 2
            rst["t"] += 1
            ht, jk, s2, tm = hts[par], junk2[par], ss2[par], tmpo[par]
            c.dma("sp", ht, ht.ap[:], h_src_ap, reads=h_src_reads)
            mb = [banks[6], banks[7]]
            for hh in range(2):
                ops = []
                for j in range(2):
                    ops.append((oat_ap[:, j * 128:(j + 1) * 128], woAB.ap[:, j, hh * 512:(hh + 1) * 512], [oat_buf, woAB]))
                for j in range(2):
                    ops.append((obt_ap_fn(j), woAB.ap[:, 2 + j, hh * 512:(hh + 1) * 512], [obt_buf, woAB]))
                for cc in range(4):
                    ops.append((oc.ap[:, cc * 128:(cc + 1) * 128], woC.ap[:, cc, hh * 512:(hh + 1) * 512], [oc, woC]))
                for oi, (l_, r_, rd) in enumerate(ops):
                    c.op("pe", lambda e, l_=l_, r_=r_, oi=oi, hh=hh: e.matmul(mb[hh].ap[:], l_, r_, start=(oi == 0), stop=(oi == 7)),
                         reads=rd, writes=[mb[hh]], nosync_same=True)
            c.op("pool", lambda e: e.memset(s2.ap[:], 0.0), writes=[s2])
            for hh in range(2):
                c.op("act", lambda e, hh=hh: e.activation(jk.ap[:, hh * 512:(hh + 1) * 512], mb[hh].ap[:], AF.Square, accum_out=s2.ap[:, hh:hh + 1]),
                     reads=[mb[hh]], writes=[jk, s2])
            c.op("dve", lambda e: e.tensor_tensor(s2.ap[:, 0:1], s2.ap[:, 0:1], s2.ap[:, 1:2], ALU.add), reads=[s2], writes=[s2])
            c.op("act", lambda e: e.activation(s2.ap[:, 0:1], s2.ap[:, 0:1], AF.Sqrt, bias=1e-6, scale=1.0 / D), reads=[s2], writes=[s2])
            c.op("dve", lambda e: e.reciprocal(s2.ap[:, 0:1], s2.ap[:, 0:1]), reads=[s2], writes=[s2])
            for hh in range(2):
                c.op("dve", lambda e, hh=hh: e.scalar_tensor_tensor(
                    tm.ap[:, hh * 512:(hh + 1) * 512], mb[hh].ap[:], s2.ap[:, 0:1], G[0][who].ap[:, hh * 512:(hh + 1) * 512], ALU.mult, ALU.mult),
                    reads=[mb[hh], s2, G[0][who]], writes=[tm])
            c.op("pool", lambda e: e.tensor_tensor(tm.ap[:], tm.ap[:], ht.ap[:], ALU.add), reads=[tm, ht], writes=[tm])
            c.dma("pool", dst_buf, dst_ap, tm.ap[:], reads=[tm], writes=[])

        n_own = NT if L == 0 else NH
        KB = 4
        tabv = tab.ap
        nkblk = n_own // KB
        ngrp = NT // TG
        bst = {"g": 0}

        def emit_B(kb, g_lo, g_hi):
            pbs = [banks[0], banks[1]]
            for gi in range(g_lo, g_hi):
                tbf = tbufs[bst["g"] % 4]
                bst["g"] += 1
                c.dma("sp", tbf, tbf.ap[:], tabv[:, gi * TG:(gi + 1) * TG, :, kb * 512:(kb + 1) * 512])
                for nci in range(TG):
                    ncx = gi * TG + nci
                    for part in range(2):
                        for f in range(2):
                            first = (ncx == 0 and part == 0)
                            lastm = (ncx == NT - 1 and part == 1)
                            c.op("pe", lambda e, tbf=tbf, nci=nci, ncx=ncx, part=part, f=f, first=first, lastm=lastm: e.matmul(
                                pbs[f].ap[:], P_all.ap[:, ncx, part * 256 + f * 128:part * 256 + (f + 1) * 128], tbf.ap[:, nci, part, :],
                                start=first, stop=lastm), reads=[P_all, tbf], writes=[pbs[f]], nosync_same=True)
            if g_hi == ngrp:
                ob = oBT[kb % 2]
                for f in range(2):
                    c.op("act", lambda e, f=f, ob=ob: e.copy(ob.ap[:, f, :], pbs[f].ap[:]), reads=[pbs[f]], writes=[ob])

        emit_B(0, 0, ngrp)
        pend2 = None
        for t in range(n_own):
            kb, ti = t // KB, t % KB
            ob = oBT[kb % 2]
            rp, rc_, rn = get_rec(t - 1), get_rec(t), get_rec(t + 1)
            mp = 2 if t == 0 else (3 if t == NH else 0)
            mn = 4 if t == NH - 1 else (5 if t == NT - 1 else 1)
            oc = attention(rc_, [(rp, mp), (rc_, None), (rn, mn), (recC[0], None), (recC[1], None)], t % 2)
            if kb + 1 < nkblk:
                emit_B(kb + 1, ti * ngrp // KB, (ti + 1) * ngrp // KB)
            if pend2 is not None:
                pend2()
            oa = oats[t % 2]
            c.dma("sp", oa, oa.ap[:], OAT.ap[t], reads=[OAT])
            src = xb.ap[t * 128:(t + 1) * 128, :] if L == 0 else H1.ap[t]

            def mk(oa=oa, ob=ob, ti=ti, oc=oc, src=src, t=t):
                return lambda: out_proj(oa, oa.ap, ob, lambda j: ob.ap[:, j, ti * 128:(ti + 1) * 128], oc, src, ([] if L == 0 else [H1]), 0, HM, HM.ap[t])
            pend2 = mk()
        pend2()
        if not last:
            obc = oBT[0]
            pbk = banks[0]
            for f in range(2):
                k_ = 0
                for ncx in range(2):
                    for part in range(2):
                        c.op("pe", lambda e, f=f, ncx=ncx, part=part, k_=k_: e.matmul(
                            pbk.ap[:, f * 256:(f + 1) * 256], P_ctx.ap[:, ncx, part * 256 + f * 128:part * 256 + (f + 1) * 128],
                            tabc_sb.ap[:, ncx, part, :], start=(k_ == 0), stop=(k_ == 3)), reads=[P_ctx, tabc_sb], writes=[pbk], nosync_same=True)
                        k_ += 1
            c.op("act", lambda e: e.copy(obc.ap[:, :, 0:256], pbk.ap[:].rearrange("p (f k) -> p f k", f=2)), reads=[pbk], writes=[obc])
            for i in range(2):
                oc = attention(recC[i], [(recC[0], None), (recC[1], None)], i)
                out_proj(oatC, oatC.ap[:, i, :], obc, lambda j, i=i: obc.ap[:, j, i * 128:(i + 1) * 128], oc,
                         ctxb.ap[i * 128:(i + 1) * 128, :], [], 1, HCM, HCM.ap[i])
        c.barrier()
        chk("pass2_%d" % L)
        st["off"] = mixer_top

        moe = (L == 1)
        nbf = [norm_bufs("p3%d" % i) for i in range(2)]
        hm4 = [sb("hm4%d" % i, [128, 4, D], F32) for i in range(1)]
        y2T = [sb("y2T%d" % i, [128, 8, 512], BF16) for i in range(1)]
        actTs = [sb("actT%d" % i, [128, FG, 512], BF16) for i in range(3)]
        wgb = [sb("wgb%d" % i, [128, 8, FG * 128], BF16) for i in range(3)]
        wub = [sb("wub%d" % i, [128, 8, FG * 128], BF16) for i in range(3)]
        wdb = [sb("wdb%d" % i, [128, FG, D], BF16) for i in range(3)]
        sg = [sb("sg%d" % i, [128, 512], F32) for i in range(2)]
        ss3 = [sb("ss3%d" % i, [128, 2], F32) for i in range(2)]
        junk3 = [sb("junk3%d" % i, [128, D], BF16) for i in range(2)]
        tm3 = [sb("tm3%d" % i, [128, D], F32) for i in range(2)]
        wst = {"g": 0, "gu": 0, "f": 0, "blk": 0}
        if moe and not SPARSE_MOE:
            acc4 = sb("acc4", [128, 4, D], F32)
            wr_sb = sb("wr_sb", [128, 8, NE], BF16)
            c.dma("pool", wr_sb, wr_sb.ap[:], w_r.ap.rearrange("(c p) n -> p c n", p=128))
            brs = sb("brs", [128, NE], F32)
            c.dma("sp", brs, brs.ap[:], b_rb.ap[:])
            lg = [sb("lg%d" % i, [128, NE], F32) for i in range(4)]
            comb = [sb("comb%d" % i, [128, NE], F32) for i in range(4)]
            rt = dict(m1=sb("rt_m1", [128, 1], F32), m2=sb("rt_m2", [128, 1], F32), k1=sb("rt_k1", [128, NE], F32),
                      k2=sb("rt_k2", [128, NE], F32), l2=sb("rt_l2", [128, NE], F32), g1=sb("rt_g1", [128, 1], F32), g2=sb("rt_g2", [128, 1], F32))

        def ffn_block(tiles_src, nt, who, dsts, wsets):
            bp = 0
            wst["blk"] += 1
            hm, yT = hm4[bp], y2T[bp]
            ntok = nt * 128
            for i, (sap, rd) in enumerate(tiles_src):
                nb = nbf[i % 2]
                c.dma("sp", nb["ht"], nb["ht"].ap[:], sap, reads=rd)
                c.op("pool", lambda e, nb=nb, i=i: e.tensor_copy(hm.ap[:, i, :], nb["ht"].ap[:]), reads=[nb["ht"]], writes=[hm])
                emit_norm_T(nb, who, 1, lambda ci, i=i: yT.ap[:, ci, i * 128:(i + 1) * 128], yT, banks[0])
                if moe:
                    lb = banks[1]
                    for ci in range(8):
                        c.op("pe", lambda e, ci=ci, i=i: e.matmul(lb.ap[:, 0:NE], yT.ap[:, ci, i * 128:(i + 1) * 128], wr_sb.ap[:, ci, :], start=(ci == 0), stop=(ci == 7)),
                             reads=[yT, wr_sb], writes=[lb], nosync_same=True)
                    l_, cb = lg[i], comb[i]
                    c.op("dve", lambda e, l_=l_: e.tensor_tensor(l_.ap[:], lb.ap[:, 0:NE], brs.ap[:], ALU.add), reads=[lb, brs], writes=[l_])
                    c.op("dve", lambda e, l_=l_: e.tensor_reduce(rt["m1"].ap[:], l_.ap[:], AX.X, ALU.max), reads=[l_], writes=[rt["m1"]])
                    c.op("dve", lambda e, l_=l_: e.tensor_scalar(rt["k1"].ap[:], l_.ap[:], rt["m1"].ap[:, 0:1], None, ALU.is_equal), reads=[l_, rt["m1"]], writes=[rt["k1"]])
                    c.op("dve", lambda e, l_=l_: e.scalar_tensor_tensor(rt["l2"].ap[:], rt["k1"].ap[:], -1e30, l_.ap[:], ALU.mult, ALU.add), reads=[l_, rt["k1"]], writes=[rt["l2"]])
                    c.op("dve", lambda e: e.tensor_reduce(rt["m2"].ap[:], rt["l2"].ap[:], AX.X, ALU.max), reads=[rt["l2"]], writes=[rt["m2"]])
                    c.op("dve", lambda e: e.tensor_scalar(rt["k2"].ap[:], rt["l2"].ap[:], rt["m2"].ap[:, 0:1], None, ALU.is_equal), reads=[rt["l2"], rt["m2"]], writes=[rt["k2"]])
                    c.op("dve", lambda e: e.tensor_tensor(rt["g2"].ap[:], rt["m2"].ap[:], rt["m1"].ap[:], ALU.subtract), reads=[rt["m1"], rt["m2"]], writes=[rt["g2"]])
                    c.op("act", lambda e: e.activation(rt["g2"].ap[:], rt["g2"].ap[:], AF.Sigmoid), reads=[rt["g2"]], writes=[rt["g2"]])
                    c.op("dve", lambda e: e.tensor_scalar(rt["g1"].ap[:], rt["g2"].ap[:], -1.0, 1.0, ALU.mult, ALU.add), reads=[rt["g2"]], writes=[rt["g1"]])
                    c.op("dve", lambda e, cb=cb: e.tensor_scalar(cb.ap[:], rt["k1"].ap[:], rt["g1"].ap[:, 0:1], None, ALU.mult), reads=[rt["k1"], rt["g1"]], writes=[cb])
                    c.op("dve", lambda e, cb=cb: e.scalar_tensor_tensor(cb.ap[:], rt["k2"].ap[:], rt["g2"].ap[:, 0:1], cb.ap[:], ALU.mult, ALU.add), reads=[rt["k2"], rt["g2"], cb], writes=[cb])
            pend_down = [None]
            for ei, (wga, wua, wda) in enumerate(wsets):
                for gi in range(NG):
                    wp = wst["g"] % 3
                    wst["g"] += 1
                    wg_, wu_, wd_ = wgb[wp], wub[wp], wdb[wp]
                    actT = actTs[wp]
                    cols = slice(gi * FG * 128, (gi + 1) * FG * 128)
                    if wga is None:
                        c.dma("sp", wg_, wg_.ap[:], WGB.ap[gi])
                        c.dma("sp", wu_, wu_.ap[:], WUB.ap[gi])
                        c.dma("sp", wd_, wd_.ap[:], WDB.ap[gi])
                    else:
                        c.dma("pool", wg_, wg_.ap[:], wga.rearrange("(c p) n -> p c n", p=128)[:, :, cols])
                        c.dma("pool", wu_, wu_.ap[:], wua.rearrange("(c p) n -> p c n", p=128)[:, :, cols])
                        c.dma("pool", wd_, wd_.ap[:], wda[gi * FG * 128:(gi + 1) * FG * 128, :].rearrange("(c p) n -> p c n", p=128))
                    for fj in range(FG):
                        j = gi * FG + fj
                        gp_ = wst["gu"] % 2
                        wst["gu"] += 1
                        gb, ub = banks[gp_ * 2], banks[gp_ * 2 + 1]
                        for ci in range(8):
                            c.op("pe", lambda e, ci=ci, fj=fj, wg_=wg_, gb=gb: e.matmul(
                                gb.ap[:, 0:ntok], wg_.ap[:, ci, fj * 128:(fj + 1) * 128], yT.ap[:, ci, 0:ntok], start=(ci == 0), stop=(ci == 7)),
                                reads=[wg_, yT], writes=[gb], nosync_same=True)
                        for ci in range(8):
                            c.op("pe", lambda e, ci=ci, fj=fj, wu_=wu_, ub=ub: e.matmul(
                                ub.ap[:, 0:ntok], wu_.ap[:, ci, fj * 128:(fj + 1) * 128], yT.ap[:, ci, 0:ntok], start=(ci == 0), stop=(ci == 7)),
                                reads=[wu_, yT], writes=[ub], nosync_same=True)
                        s_ = sg[gp_]
                        c.op("act", lambda e, gb=gb, s_=s_: e.activation(s_.ap[:, 0:ntok], gb.ap[:, 0:ntok], AF.Silu), reads=[gb], writes=[s_])
                        c.op("dve", lambda e, ub=ub, s_=s_, fj=fj, actT=actT: e.tensor_tensor(actT.ap[:, fj, 0:ntok], ub.ap[:, 0:ntok], s_.ap[:, 0:ntok], ALU.mult),
                             reads=[ub, s_], writes=[actT])
                    def down(gi=gi, wd_=wd_, actT=actT):
                        for i in range(nt):
                            fp_ = wst["f"] % 2
                            wst["f"] += 1
                            fb_ = [banks[4 + fp_ * 2], banks[5 + fp_ * 2]]
                            for hh in range(2):
                                for fj in range(FG):
                                    c.op("pe", lambda e, i=i, hh=hh, fj=fj, wd_=wd_, fb_=fb_, actT=actT: e.matmul(
                                        fb_[hh].ap[:], actT.ap[:, fj, i * 128:(i + 1) * 128], wd_.ap[:, fj, hh * 512:(hh + 1) * 512],
                                        start=(fj == 0), stop=(fj == FG - 1)), reads=[actT, wd_], writes=[fb_[hh]], nosync_same=True)
                            for hh in range(2):
                                dst = facc.ap[:, i, hh * 512:(hh + 1) * 512]
                                if gi == 0:
                                    c.op("act", lambda e, dst=dst, hh=hh, fb_=fb_: e.copy(dst, fb_[hh].ap[:]), reads=[fb_[hh]], writes=[facc])
                                else:
                                    c.op("dve", lambda e, dst=dst, hh=hh, fb_=fb_: e.tensor_tensor(dst, fb_[hh].ap[:], dst, ALU.add), reads=[fb_[hh], facc], writes=[facc])
                    if pend_down[0] is not None:
                        pend_down[0]()
                    pend_down[0] = down
                pend_down[0]()
                pend_down[0] = None
                if moe:
                    for i in range(nt):
                        for hh in range(2):
                            dst = acc4.ap[:, i, hh * 512:(hh + 1) * 512]
                            srcf = facc.ap[:, i, hh * 512:(hh + 1) * 512]
                            if ei == 0:
                                c.op("dve", lambda e, dst=dst, srcf=srcf, i=i, ei=ei: e.tensor_scalar(dst, srcf, comb[i].ap[:, ei:ei + 1], None, ALU.mult),
                                     reads=[facc, comb[i]], writes=[acc4])
                            else:
                                c.op("dve", lambda e, dst=dst, srcf=srcf, i=i, ei=ei: e.scalar_tensor_tensor(dst, srcf, comb[i].ap[:, ei:ei + 1], dst, ALU.mult, ALU.add),
                                     reads=[facc, comb[i], acc4], writes=[acc4])
            fin = acc4 if moe else facc
            for i in range(nt):
                p_ = i % 2
                s3, jk, tm = ss3[p_], junk3[p_], tm3[p_]
                c.op("pool", lambda e, s3=s3: e.memset(s3.ap[:], 0.0), writes=[s3])
                c.op("act", lambda e, i=i, s3=s3, jk=jk: e.activation(jk.ap[:], fin.ap[:, i, :], AF.Square, accum_out=s3.ap[:, 0:1]), reads=[fin], writes=[jk, s3])
                c.op("act", lambda e, s3=s3: e.activation(s3.ap[:, 0:1], s3.ap[:, 0:1], AF.Sqrt, bias=1e-6, scale=1.0 / D), reads=[s3], writes=[s3])
                c.op("dve", lambda e, s3=s3: e.reciprocal(s3.ap[:, 0:1], s3.ap[:, 0:1]), reads=[s3], writes=[s3])
                c.op("dve", lambda e, i=i, s3=s3, tm=tm: e.scalar_tensor_tensor(tm.ap[:], fin.ap[:, i, :], s3.ap[:, 0:1], G[1][who].ap[:], ALU.mult, ALU.mult),
                     reads=[fin, s3, G[1][who]], writes=[tm])
                c.op("pool", lambda e, i=i, tm=tm: e.tensor_tensor(tm.ap[:], tm.ap[:], hm.ap[:, i, :], ALU.add), reads=[tm, hm], writes=[tm])
                dbuf, dap = dsts[i]
                c.dma("sp", dbuf, dap, tm.ap[:], reads=[tm], writes=[])


        def idma(gather, out_ap, in_ap, idx_ap, reads, writes):
            def fn(e):
                if gather:
                    return e.indirect_dma_start(out=out_ap, out_offset=None, in_=in_ap,
                                                in_offset=bass.IndirectOffsetOnAxis(ap=idx_ap, axis=0))
                return e.indirect_dma_start(out=out_ap, out_offset=bass.IndirectOffsetOnAxis(ap=idx_ap, axis=0),
                                            in_=in_ap, in_offset=None)
            c.dma_custom("pool", fn, reads, writes)

        def moe_sparse():
            st["off"] = mixer_top
            base = st["off"]
            K1 = sb("K1", [128, NHt, NE], F32)
            K2 = sb("K2", [128, NHt, NE], F32)
            POS = sb("POS", [128, NHt, NE], F32)
            G1 = sb("G1", [128, NHt], F32)
            G2 = sb("G2", [128, NHt], F32)
            P1f = sb("P1f", [128, NHt], F32)
            P2f = sb("P2f", [128, NHt], F32)
            IDX1 = sb("IDX1", [128, NHt], I32)
            IDX2 = sb("IDX2", [128, NHt], I32)
            carry = sb("carry", [128, NE], F32)
            offv = sb("offv", [128, NE], F32)
            endv = sb("endv", [128, NE], F32)
            npf = sb("npf", [128, NE], F32)
            Es = sb("Es", [128, NS], F32)
            EsW = sb("EsW", [128, NS], F32)
            EsD = sb("EsD", [128, NS], F32)
            ltri = sb("ltri", [128, 128], BF16)
            onesb = sb("onesb", [128, 128], BF16)
            cW = sb("cW", [128, 8 * NG], F32)
            cD = sb("cD", [128, NFC], F32)
            wr_sb = sb("wr_sb", [128, 8, NE], BF16)
            brs = sb("brs", [128, NE], F32)
            c.dma("sp", ltri, ltri.ap[:], ltrid.ap[:])
            c.dma("sp", cW, cW.ap[:], constWd.ap[:])
            c.dma("sp", cD, cD.ap[:], constDd.ap[:])
            c.dma("pool", wr_sb, wr_sb.ap[:], w_r.ap.rearrange("(c p) n -> p c n", p=128))
            c.dma("sp", brs, brs.ap[:], b_rb.ap[:])
            c.op("dve", lambda e: e.memset(onesb.ap[:], 1.0), writes=[onesb])
            c.op("dve", lambda e: e.memset(carry.ap[:], 0.0), writes=[carry])
            state_end = st["off"]

            nbr = [norm_bufs("r1%d" % i) for i in range(2)]
            yTr = [sb("yTr%d" % i, [128, 8, 128], BF16) for i in range(2)]
            tmpf = [sb("tmpf%d" % i, [128, D], F32) for i in range(2)]
            y2t = [sb("y2t%d" % i, [128, D], BF16) for i in range(2)]
            lgt = sb("lgt", [128, NE], F32)
            l2t = sb("l2t", [128, NE], F32)
            m1 = sb("m1", [128, 1], F32)
            m2 = sb("m2", [128, 1], F32)
            Mf = sb("Mf", [128, NE], F32)
            Mb = [sb("Mb%d" % i, [128, NE], BF16) for i in range(2)]
            for i in range(NHt):
                p = i % 2
                nb, yT = nbr[p], yTr[p]
                c.dma("sp", nb["ht"], nb["ht"].ap[:], HM.ap[i], reads=[HM])
                emit_norm_T(nb, 0, 1, lambda ci, yT=yT: yT.ap[:, ci, :], yT, banks[0])
                tf, yt = tmpf[p], y2t[p]
                c.op("dve", lambda e, nb=nb, tf=tf: e.tensor_tensor(tf.ap[:], nb["xn"].ap[:], MULbc.ap[:], ALU.mult), reads=[nb["xn"], MULbc], writes=[tf])
                c.op("pool", lambda e, tf=tf, yt=yt: e.tensor_tensor(yt.ap[:], tf.ap[:], ADDbc.ap[:], ALU.add), reads=[tf, ADDbc], writes=[yt])
                c.dma("pool", Y2, Y2.ap[i], yt.ap[:], reads=[yt], writes=[])
                lb = banks[1]
                for ci in range(8):
                    c.op("pe", lambda e, ci=ci, yT=yT: e.matmul(lb.ap[:, 0:NE], yT.ap[:, ci, :], wr_sb.ap[:, ci, :], start=(ci == 0), stop=(ci == 7)),
                         reads=[yT, wr_sb], writes=[lb], nosync_same=True)
                k1, k2 = K1.ap[:, i, :], K2.ap[:, i, :]
                g1, g2 = G1.ap[:, i:i + 1], G2.ap[:, i:i + 1]
                c.op("dve", lambda e: e.tensor_tensor(lgt.ap[:], lb.ap[:, 0:NE], brs.ap[:], ALU.add), reads=[lb, brs], writes=[lgt])
                c.op("dve", lambda e: e.tensor_reduce(m1.ap[:], lgt.ap[:], AX.X, ALU.max), reads=[lgt], writes=[m1])
                c.op("dve", lambda e, k1=k1: e.tensor_scalar(k1, lgt.ap[:], m1.ap[:, 0:1], None, ALU.is_equal), reads=[lgt, m1], writes=[K1])
                c.op("dve", lambda e, k1=k1: e.scalar_tensor_tensor(l2t.ap[:], k1, -1e30, lgt.ap[:], ALU.mult, ALU.add), reads=[lgt, K1], writes=[l2t])
                c.op("dve", lambda e: e.tensor_reduce(m2.ap[:], l2t.ap[:], AX.X, ALU.max), reads=[l2t], writes=[m2])
                c.op("dve", lambda e, k2=k2: e.tensor_scalar(k2, l2t.ap[:], m2.ap[:, 0:1], None, ALU.is_equal), reads=[l2t, m2], writes=[K2])
                c.op("dve", lambda e, g2=g2: e.tensor_tensor(g2, m2.ap[:], m1.ap[:], ALU.subtract), reads=[m1, m2], writes=[G2])
                c.op("act", lambda e, g2=g2: e.activation(g2, g2, AF.Sigmoid), reads=[G2], writes=[G2])
                c.op("dve", lambda e, g1=g1, g2=g2: e.tensor_scalar(g1, g2, -1.0, 1.0, ALU.mult, ALU.add), reads=[G2], writes=[G1])
                mb_ = Mb[p]
                c.op("dve", lambda e, k1=k1, k2=k2, mb_=mb_: e.tensor_tensor(mb_.ap[:], k1, k2, ALU.add), reads=[K1, K2], writes=[mb_])
                pb_ = banks[2]
                c.op("pe", lambda e, mb_=mb_: e.matmul(pb_.ap[:, 0:NE], ltri.ap[:], mb_.ap[:], start=True, stop=True), reads=[ltri, mb_], writes=[pb_], nosync_same=True)
                c.op("pe", lambda e, mb_=mb_: e.matmul(pb_.ap[:, NE:2 * NE], onesb.ap[:], mb_.ap[:], start=True, stop=True), reads=[onesb, mb_], writes=[pb_], nosync_same=True)
                c.op("dve", lambda e, i=i: e.tensor_tensor(POS.ap[:, i, :], pb_.ap[:, 0:NE], carry.ap[:], ALU.add), reads=[pb_, carry], writes=[POS])
                c.op("dve", lambda e: e.tensor_tensor(carry.ap[:], pb_.ap[:, NE:2 * NE], carry.ap[:], ALU.add), reads=[pb_, carry], writes=[carry])
            c.barrier()
            tq = sb("tq", [128, NE], F32)
            c.op("dve", lambda e: e.memset(npf.ap[:], 0.0), writes=[npf])
            for j in range(T_OWN // 512):
                c.op("dve", lambda e, j=j: e.tensor_scalar(tq.ap[:], carry.ap[:], float(512 * j), None, ALU.is_gt), reads=[carry], writes=[tq])
                c.op("dve", lambda e: e.tensor_tensor(npf.ap[:], npf.ap[:], tq.ap[:], ALU.add), reads=[npf, tq], writes=[npf])
            c.op("dve", lambda e: e.tensor_scalar(npf.ap[:], npf.ap[:], 512.0, None, ALU.mult), reads=[npf], writes=[npf])
            c.op("dve", lambda e: e.memset(offv.ap[:], 0.0), writes=[offv])
            for e_ in range(1, NE):
                c.op("dve", lambda e, e_=e_: e.tensor_tensor(offv.ap[:, e_:e_ + 1], offv.ap[:, e_ - 1:e_], npf.ap[:, e_ - 1:e_], ALU.add),
                     reads=[offv, npf], writes=[offv])
            c.op("dve", lambda e: e.tensor_tensor(endv.ap[:], offv.ap[:], npf.ap[:], ALU.add), reads=[offv, npf], writes=[endv])
            for i in range(NHt):
                for Kx, Px in ((K1, P1f), (K2, P2f)):
                    c.op("dve", lambda e, i=i: e.tensor_tensor(tq.ap[:], POS.ap[:, i, :], offv.ap[:], ALU.add), reads=[POS, offv], writes=[tq])
                    c.op("dve", lambda e, i=i, Kx=Kx: e.tensor_tensor(tq.ap[:], tq.ap[:], Kx.ap[:, i, :], ALU.mult), reads=[tq, Kx], writes=[tq])
                    c.op("dve", lambda e, i=i, Px=Px: e.tensor_reduce(Px.ap[:, i:i + 1], tq.ap[:], AX.X, ALU.add), reads=[tq], writes=[Px])
            c.op("dve", lambda e: e.tensor_copy(IDX1.ap[:], P1f.ap[:]), reads=[P1f], writes=[IDX1])
            c.op("dve", lambda e: e.tensor_copy(IDX2.ap[:], P2f.ap[:]), reads=[P2f], writes=[IDX2])
            for s_ in range(NS):
                c.op("dve", lambda e, s_=s_: e.tensor_scalar(tq.ap[:], endv.ap[:], float(512 * s_), None, ALU.is_le), reads=[endv], writes=[tq])
                c.op("dve", lambda e, s_=s_: e.tensor_reduce(Es.ap[:, s_:s_ + 1], tq.ap[:], AX.X, ALU.add), reads=[tq], writes=[Es])
            c.op("dve", lambda e: e.tensor_scalar(Es.ap[:], Es.ap[:], float(NE - 1), None, ALU.min), reads=[Es], writes=[Es])
            c.op("dve", lambda e: e.tensor_scalar(EsW.ap[:], Es.ap[:], float(1024 * RPD), None, ALU.mult), reads=[Es], writes=[EsW])
            c.op("dve", lambda e: e.tensor_scalar(EsD.ap[:], Es.ap[:], float(DFF), None, ALU.mult), reads=[Es], writes=[EsD])
            for i in range(NHt):
                yt = y2t[i % 2]
                c.dma("sp", yt, yt.ap[:], Y2.ap[i], reads=[Y2])
                idma(False, Y2S.ap[:, :], yt.ap[:], IDX1.ap[:, i:i + 1], [yt, IDX1], [])
                idma(False, Y2S.ap[:, :], yt.ap[:], IDX2.ap[:, i:i + 1], [yt, IDX2], [])
            c.barrier()
            st["off"] = state_end
            ytm = [sb("ytm%d" % i, [128, D], BF16) for i in range(2)]
            yTs = [sb("yTs%d" % i, [128, 8, 512], BF16) for i in range(2)]
            idxWf = sb("idxWf", [128, 8 * NG], F32)
            idxDf = sb("idxDf", [128, NFC], F32)
            idxW = [sb("idxW%d" % i, [128, 8 * NG], I32) for i in range(2)]
            idxD = [sb("idxD%d" % i, [128, NFC], I32) for i in range(2)]
            NWS = 3
            wgs = [sb("wgs%d" % i, [128, 8, 512], BF16) for i in range(NWS)]
            wus = [sb("wus%d" % i, [128, 8, 512], BF16) for i in range(NWS)]
            wds = [sb("wds%d" % i, [128, FG, D], BF16) for i in range(NWS)]
            acts = [sb("acts%d" % i, [128, FG, 512], BF16) for i in range(NWS)]
            sgs = [sb("sgs%d" % i, [128, 512], F32) for i in range(2)]
            faccs = [sb("faccs%d" % i, [128, 4, D], F32) for i in range(2)]
            wgc = [[Buf("wgc", w_.ap[:, ci, :]) for ci in range(8)] for w_ in wgs]
            wuc = [[Buf("wuc", w_.ap[:, ci, :]) for ci in range(8)] for w_ in wus]
            wdc = [[Buf("wdc", w_.ap[:, fj, :]) for fj in range(FG)] for w_ in wds]
            wgf = wg_e.ap.rearrange("e d (r n) -> (e d r) n", n=512)
            wuf = wu_e.ap.rearrange("e d (r n) -> (e d r) n", n=512)
            wdf = wd_e.ap.rearrange("e f n -> (e f) n")
            sst = {"g": 0, "gu": 0, "f": 0}
            spend = [None]
            def slot_front(s_):
                sp_ = s_ % 2
                iw, idd, yT = idxW[sp_], idxD[sp_], yTs[sp_]
                c.op("dve", lambda e, s_=s_: e.tensor_scalar(idxWf.ap[:], cW.ap[:], EsW.ap[:, s_:s_ + 1], None, ALU.add), reads=[cW, EsW], writes=[idxWf])
                c.op("dve", lambda e, iw=iw: e.tensor_copy(iw.ap[:], idxWf.ap[:]), reads=[idxWf], writes=[iw])
                c.op("dve", lambda e, s_=s_: e.tensor_scalar(idxDf.ap[:], cD.ap[:], EsD.ap[:, s_:s_ + 1], None, ALU.add), reads=[cD, EsD], writes=[idxDf])
                c.op("dve", lambda e, idd=idd: e.tensor_copy(idd.ap[:], idxDf.ap[:]), reads=[idxDf], writes=[idd])
                for i in range(4):
                    ym = ytm[i % 2]
                    c.dma("sp", ym, ym.ap[:], Y2S.ap[s_ * 512 + i * 128:s_ * 512 + (i + 1) * 128, :], reads=[Y2S])
                    tb = banks[0]
                    tvv = bfv(tb)
                    for ci in range(8):
                        c.op("pe", lambda e, ci=ci, ym=ym: e.transpose(tvv[:, ci * 128:(ci + 1) * 128], ym.ap[:, ci * 128:(ci + 1) * 128], ident_b.ap[:]),
                             reads=[ym, ident_b], writes=[tb], nosync_same=True)
                    c.op("act", lambda e, i=i, yT=yT: e.copy(yT.ap[:, :, i * 128:(i + 1) * 128], tvv.rearrange("p (c t) -> p c t", t=128)),
                         reads=[tb], writes=[yT])
            slot_front(0)
            for s_ in range(NS):
                sp_ = s_ % 2
                iw, idd, yT, fa = idxW[sp_], idxD[sp_], yTs[sp_], faccs[sp_]
                for gi in range(NG):
                    wp = sst["g"] % NWS
                    sst["g"] += 1
                    wg_, wu_, wd_, actT = wgs[wp], wus[wp], wds[wp], acts[wp]
                    for ci in range(8):
                        idma(True, wg_.ap[:, ci, :], wgf, iw.ap[:, ci * NG + gi:ci * NG + gi + 1], [iw], [wgc[wp][ci]])
                    for ci in range(8):
                        idma(True, wu_.ap[:, ci, :], wuf, iw.ap[:, ci * NG + gi:ci * NG + gi + 1], [iw], [wuc[wp][ci]])
                    for fj in range(FG):
                        k_ = gi * FG + fj
                        idma(True, wd_.ap[:, fj, :], wdf, idd.ap[:, k_:k_ + 1], [idd], [wdc[wp][fj]])
                    for fj in range(FG):
                        gp_ = sst["gu"] % 2
                        sst["gu"] += 1
                        gb, ub = banks[gp_ * 2], banks[gp_ * 2 + 1]
                        for ci in range(8):
                            c.op("pe", lambda e, ci=ci, fj=fj, wg_=wg_, gb=gb, yT=yT: e.matmul(
                                gb.ap[:], wg_.ap[:, ci, fj * 128:(fj + 1) * 128], yT.ap[:, ci, :], start=(ci == 0), stop=(ci == 7)),
                                reads=[wgc[wp][ci], yT], writes=[gb], nosync_same=True)
                        for ci in range(8):
                            c.op("pe", lambda e, ci=ci, fj=fj, wu_=wu_, ub=ub, yT=yT: e.matmul(
                                ub.ap[:], wu_.ap[:, ci, fj * 128:(fj + 1) * 128], yT.ap[:, ci, :], start=(ci == 0), stop=(ci == 7)),
                                reads=[wuc[wp][ci], yT], writes=[ub], nosync_same=True)
                        sg_ = sgs[gp_]
                        c.op("act", lambda e, gb=gb, sg_=sg_: e.activation(sg_.ap[:], gb.ap[:], AF.Silu), reads=[gb], writes=[sg_])
                        c.op("dve", lambda e, ub=ub, sg_=sg_, fj=fj, actT=actT: e.tensor_tensor(actT.ap[:, fj, :], ub.ap[:], sg_.ap[:], ALU.mult),
                             reads=[ub, sg_], writes=[actT])
                    def sdown(gi=gi, wp=wp, wd_=wd_, actT=actT, fa=fa):
                        for i in range(4):
                            fp_ = sst["f"] % 2
                            sst["f"] += 1
                            fb_ = [banks[4 + fp_ * 2], banks[5 + fp_ * 2]]
                            for hh in range(2):
                                for fj in range(FG):
                                    c.op("pe", lambda e, i=i, hh=hh, fj=fj, wd_=wd_, fb_=fb_, actT=actT: e.matmul(
                                        fb_[hh].ap[:], actT.ap[:, fj, i * 128:(i + 1) * 128], wd_.ap[:, fj, hh * 512:(hh + 1) * 512],
                                        start=(fj == 0), stop=(fj == FG - 1)), reads=[actT, wdc[wp][fj]], writes=[fb_[hh]], nosync_same=True)
                            for hh in range(2):
                                dst = fa.ap[:, i, hh * 512:(hh + 1) * 512]
                                if gi == 0:
                                    c.op("act", lambda e, dst=dst, hh=hh, fb_=fb_: e.copy(dst, fb_[hh].ap[:]), reads=[fb_[hh]], writes=[fa])
                                else:
                                    c.op("dve", lambda e, dst=dst, hh=hh, fb_=fb_: e.tensor_tensor(dst, fb_[hh].ap[:], dst, ALU.add), reads=[fb_[hh], fa], writes=[fa])
                    if spend[0] is not None:
                        spend[0]()
                    spend[0] = sdown
                    if gi == max(0, NG - 2) and s_ + 1 < NS:
                        slot_front(s_ + 1)
                spend[0]()
                spend[0] = None
                for i in range(4):
                    c.dma("sp", RS, RS.ap[s_ * 512 + i * 128:s_ * 512 + (i + 1) * 128, :], fa.ap[:, i, :], reads=[fa], writes=[])
            c.barrier()
            st["off"] = state_end
            r1 = [sb("r1t%d" % i, [128, D], F32) for i in range(2)]
            r2 = [sb("r2t%d" % i, [128, D], F32) for i in range(2)]
            hmt = [sb("hmt%d" % i, [128, D], F32) for i in range(2)]
            jk3 = [sb("jk3%d" % i, [128, D], BF16) for i in range(2)]
            s3s = [sb("s3s%d" % i, [128, 2], F32) for i in range(2)]
            for i in range(NHt):
                p = i % 2
                a, b, hm_, jk, s3 = r1[p], r2[p], hmt[p], jk3[p], s3s[p]
                idma(True, a.ap[:], RS.ap[:, :], IDX1.ap[:, i:i + 1], [IDX1, RS], [a])
                idma(True, b.ap[:], RS.ap[:, :], IDX2.ap[:, i:i + 1], [IDX2, RS], [b])
                c.dma("sp", hm_, hm_.ap[:], HM.ap[i], reads=[HM])
                c.op("dve", lambda e, a=a, i=i: e.tensor_scalar(a.ap[:], a.ap[:], G1.ap[:, i:i + 1], None, ALU.mult), reads=[a, G1], writes=[a])
                c.op("dve", lambda e, a=a, b=b, i=i: e.scalar_tensor_tensor(a.ap[:], b.ap[:], G2.ap[:, i:i + 1], a.ap[:], ALU.mult, ALU.add), reads=[a, b, G2], writes=[a])
                c.op("dve", lambda e, s3=s3: e.memset(s3.ap[:], 0.0), writes=[s3])
                c.op("act", lambda e, a=a, jk=jk, s3=s3: e.activation(jk.ap[:], a.ap[:], AF.Square, accum_out=s3.ap[:, 0:1]), reads=[a], writes=[jk, s3])
                c.op("act", lambda e, s3=s3: e.activation(s3.ap[:, 0:1], s3.ap[:, 0:1], AF.Sqrt, bias=1e-6, scale=1.0 / D), reads=[s3], writes=[s3])
                c.op("dve", lambda e, s3=s3: e.reciprocal(s3.ap[:, 0:1], s3.ap[:, 0:1]), reads=[s3], writes=[s3])
                c.op("dve", lambda e, a=a, b=b, s3=s3: e.scalar_tensor_tensor(b.ap[:], a.ap[:], s3.ap[:, 0:1], G[1][0].ap[:], ALU.mult, ALU.mult),
                     reads=[a, s3, G[1][0]], writes=[b])
                c.op("dve", lambda e, b=b, hm_=hm_: e.tensor_tensor(b.ap[:], b.ap[:], hm_.ap[:], ALU.add), reads=[b, hm_], writes=[b])
                c.dma("sp", out, out.ap[i * 128:(i + 1) * 128, :], b.ap[:], reads=[b], writes=[])
            st["off"] = base

        facc = sb("facc", [128, 4, D], F32)
        if moe and SPARSE_MOE:
            moe_sparse()
        elif not moe:
            wset = [(None, None, None)] if PRECONV else [(wg_d.ap, wu_d.ap, wd_d.ap)]
            for b_ in range(NT // 4):
                ts = [(HM.ap[b_ * 4 + i], [HM]) for i in range(4)]
                ds = [(H1, H1.ap[b_ * 4 + i]) for i in range(4)]
                ffn_block(ts, 4, 0, ds, wset)
            ts = [(HCM.ap[i], [HCM]) for i in range(2)]
            ds = [(HC1, HC1.ap[i]) for i in range(2)]
            ffn_block(ts, 2, 1, ds, wset)
        else:
            wset = [(wg_e.ap[e_], wu_e.ap[e_], wd_e.ap[e_]) for e_ in range(NE)]
            for b_ in range(NH // 4):
                ts = [(HM.ap[b_ * 4 + i], [HM]) for i in range(4)]
                ds = [(out, out.ap[(b_ * 4 + i) * 128:(b_ * 4 + i + 1) * 128, :]) for i in range(4)]
                ffn_block(ts, 4, 0, ds, wset)
        c.barrier()

    try:
        chk("init")
        layer(0)
        chk("layer0")
        layer(1)
    except _Stop:
        pass
    c.finish()
    return nc


_CONST_CACHE = {}


def _consts(cfg, s):
    key = (cfg.SEQ, s)
    if key in _CONST_CACHE:
        return _CONST_CACHE[key]
    SEQ, NT, NH = cfg.SEQ, cfg.NT, cfg.NH
    bf = ml_dtypes.bfloat16
    g = (np.arange(SEQ, dtype=np.int64) + s * (SEQ // 2)) % SEQ
    inv = (10000.0 ** (-(np.arange(0, 32, 2, dtype=np.float32) / 32.0))).astype(np.float32)
    row = (g // 64).astype(np.float32)
    col = (g % 64).astype(np.float32)
    ang = np.concatenate([row[:, None] * inv[None, :], col[:, None] * inv[None, :]], axis=1).astype(np.float32)
    rc = np.ones((NT + 1, 128, 32), np.float32)
    rs = np.zeros((NT + 1, 128, 32), np.float32)
    rc[:NT] = np.cos(ang).reshape(NT, 128, 32)
    rs[:NT] = np.sin(ang).reshape(NT, 128, 32)
    m = (g[:, None].astype(np.int32) * g[None, :].astype(np.int32)) % SEQ
    k = np.arange(SEQ, dtype=np.float64)
    sc = 1.0 / np.sqrt(SEQ * 64.0)
    lc = (np.cos(2 * np.pi * k / SEQ) * sc).astype(np.float32).astype(bf)
    ls = (-np.sin(2 * np.pi * k / SEQ) * sc).astype(np.float32).astype(bf)
    tab = np.empty((128, NT, 2, SEQ), bf)
    tab[:, :, 0, :] = lc[m].reshape(NT, 128, SEQ).transpose(1, 0, 2)
    tab[:, :, 1, :] = ls[m].reshape(NT, 128, SEQ).transpose(1, 0, 2)
    del m
    j = np.arange(128)[:, None]
    i = np.arange(128)[None, :]
    prev = (j >= i).astype(np.float32)
    nxt = (j <= i).astype(np.float32)
    z = np.zeros((128, 128), np.float32)
    if s == 0:
        ms = [prev, nxt, z, prev, nxt, z]
    else:
        ms = [prev, nxt, prev, z, z, nxt]
    masks = np.stack(ms, axis=1).astype(bf)
    out = dict(ropec=rc, ropes=rs, tab=tab, masks=masks)
    _CONST_CACHE[key] = out
    return out


def _shared_consts():
    if "shared" in _CONST_CACHE:
        return _CONST_CACHE["shared"]
    bf = ml_dtypes.bfloat16
    n = np.arange(256, dtype=np.float64)
    a = 2 * np.pi * ((n[:, None] * n[None, :]) % 256) / 256.0
    sc = 1.0 / np.sqrt(256 * 64.0)
    tc = np.empty((128, 2, 2, 256), bf)
    tc[:, :, 0, :] = (np.cos(a) * sc).astype(np.float32).reshape(2, 128, 256).transpose(1, 0, 2).astype(bf)
    tc[:, :, 1, :] = (-np.sin(a) * sc).astype(np.float32).reshape(2, 128, 256).transpose(1, 0, 2).astype(bf)
    d = np.arange(64, dtype=np.float64)
    a64 = 2 * np.pi * ((d[:, None] * d[None, :]) % 64) / 64.0
    c64 = np.zeros((128, 128), np.float32)
    s64 = np.zeros((128, 128), np.float32)
    for gI in range(2):
        c64[gI * 64:(gI + 1) * 64, gI * 64:(gI + 1) * 64] = np.cos(a64)
        s64[gI * 64:(gI + 1) * 64, gI * 64:(gI + 1) * 64] = np.sin(a64)
    tp_ = np.arange(128)
    ltri = (tp_[:, None] < tp_[None, :]).astype(np.float32).astype(bf)
    out = dict(tabc=tc, c64bd=c64, s64bd=s64, ident_b=np.eye(128, dtype=np.float32).astype(bf),
               ident_f=np.eye(128, dtype=np.float32), ltri=ltri)
    _CONST_CACHE["shared"] = out
    return out


def prep(inp, cfg):
    f32 = np.float32
    A = lambda k_: np.ascontiguousarray(np.asarray(inp[k_], dtype=f32))
    x, cc, ctx, c_ctx = A("x"), A("c"), A("ctx"), A("c_ctx")
    SEQ = cfg.SEQ
    sh = dict(_shared_consts())
    NGh, RPDh, NFCh = cfg.NFC // cfg.FG, cfg.D_FF // 512, cfg.NFC
    pp = np.arange(128)[:, None]
    cW = np.zeros((128, 8 * NGh), np.float32)
    for ci in range(8):
        for g_ in range(NGh):
            cW[:, ci * NGh + g_] = (ci * 128 + pp[:, 0]) * RPDh + g_
    cD = np.zeros((128, NFCh), np.float32)
    for j in range(NFCh):
        cD[:, j] = j * 128 + pp[:, 0]
    sh["constW"], sh["constD"] = cW, cD
    sh["w_ada"] = A("w_ada")
    sh["badaT"] = np.ascontiguousarray(A("b_ada").reshape(2, 48, 128).transpose(0, 2, 1))
    gm, gf = A("g_mix_pre"), A("g_ffn_pre")
    gpreT = np.stack([gm.reshape(2, 8, 128), gf.reshape(2, 8, 128)], axis=1)
    sh["gpreT"] = np.ascontiguousarray(gpreT.transpose(0, 3, 1, 2))
    gpost = np.stack([A("g_mix_post"), A("g_ffn_post")], axis=1)
    sh["gpost"] = np.ascontiguousarray(np.broadcast_to(gpost[:, :, None, :], (2, 2, 128, 1024)))
    w_in = A("w_in").copy()
    q = w_in[:, :, 768:1280].reshape(2, 1024, 8, 64)
    order = [0, 4, 1, 5, 2, 6, 3, 7]
    w_in[:, :, 768:1280] = q[:, :, order, :].reshape(2, 1024, 512)
    sh["w_in"] = w_in
    sh["wsT"] = np.ascontiguousarray(A("w_s").transpose(0, 1, 3, 2))
    sh["bsT"] = np.ascontiguousarray(A("b_s").transpose(0, 2, 1))
    sh["gvb"] = np.ascontiguousarray(np.broadcast_to(A("g_v").reshape(2, 1, 256), (2, 128, 256)))
    sh["wf"] = A("w_f").reshape(2, 256, 64)
    sh["sinkb"] = np.ascontiguousarray(np.broadcast_to(A("sink").reshape(2, 1, 8), (2, 128, 8)))
    sh["w_out"] = A("w_out")
    sh["wg_d"], sh["wu_d"], sh["wd_d"] = A("w_gate_d")[0], A("w_up_d")[0], A("w_down_d")[0]
    sh["w_r"] = A("w_router")[0]
    sh["b_rb"] = np.ascontiguousarray(np.broadcast_to(A("b_router")[0].reshape(1, 8), (128, 8)))
    sh["wg_e"], sh["wu_e"], sh["wd_e"] = A("w_gate_e")[0], A("w_up_e")[0], A("w_down_e")[0]
    maps = []
    for core in range(cfg.NCORES):
        b, s = core // 2, core % 2
        m = dict(sh)
        m.update(_consts(cfg, s))
        m["xb"] = np.ascontiguousarray(np.roll(x[b], -s * (SEQ // 2), axis=0))
        m["ctxb"] = np.ascontiguousarray(ctx[b])
        cv = np.stack([cc[b].reshape(8, 128), c_ctx.reshape(8, 128)], axis=-1)
        m["cvecT"] = np.ascontiguousarray(cv.transpose(1, 0, 2))
        maps.append(m)
    return maps


_NC_CACHE = {}


def run(inp, cfg):
    key = (cfg.SEQ, cfg.D_FF, cfg.BATCH)
    if key not in _NC_CACHE:
        _NC_CACHE[key] = build(cfg)
    nc = _NC_CACHE[key]
    maps = prep(inp, cfg)
    res = run_bass_kernel_spmd(nc, maps, core_ids=list(range(cfg.NCORES)))
    SEQ = cfg.SEQ
    outp = np.empty((cfg.BATCH, SEQ, 1024), np.float32)
    for core in range(cfg.NCORES):
        b, s = core // 2, core % 2
        outp[b, s * (SEQ // 2):(s + 1) * (SEQ // 2)] = res.results[core]["out"]
    return outp


def kernel(**inputs):
    return run(inputs, Cfg())
```
